# Optimizing a Trainium2 kernel written in Bass

```python
import jax, jax.numpy as jnp
from jax import lax
import numpy as np

D_MODEL = 1024
BATCH = 8
SEQ = 8192
DEPTH = 4

N_MIXERS = 4
RMS_EPS = 1e-6
N_MOD = 6

SB_HEADS = 16
SB_HEAD_DIM = D_MODEL // SB_HEADS
SB_QBLOCK = 128

GLA_HEADS = 4
GLA_DK = D_MODEL // (2 * GLA_HEADS)
GLA_DV = D_MODEL // GLA_HEADS
GLA_GATE_RANK = 16
GLA_GATE_TAU = 16.0
GLA_CHUNK = 64
GLA_IN = 2 * GLA_HEADS * GLA_DK + 2 * GLA_HEADS * GLA_DV + GLA_GATE_RANK

CONV_WIDTH = 3

DIL_PATTERNS = ((128, 1), (512, 4), (2048, 16))
DIL_GROUPS = len(DIL_PATTERNS)
DIL_HEADS = 8
DIL_HEAD_DIM = 64
DIL_QBLOCK = 128

MOE_GROUPS = 4
MOE_PER_GROUP = 8
MOE_EXPERTS = MOE_GROUPS * MOE_PER_GROUP
MOE_TOPK = 2
MOE_D_FF = D_MODEL // 2
MOE_BLOCK = 128

N_SB = (DEPTH + 3) // N_MIXERS
N_GLA = (DEPTH + 2) // N_MIXERS
N_CONV = (DEPTH + 1) // N_MIXERS
N_DIL = DEPTH // N_MIXERS

kernel_name = "hybrid_sb_gla_conv_dilated_hmoe"


def rms_norm(x, g):
    x32 = x.astype(jnp.float32)
    y = x32 * lax.rsqrt(jnp.mean(x32 * x32, axis=-1, keepdims=True) + RMS_EPS)
    return (y * g.astype(jnp.float32)).astype(x.dtype)


def stick_breaking_attention(q, k, v):
    B, H, S, d = q.shape
    nb = S // SB_QBLOCK
    scale = d ** -0.5
    kpos = jnp.arange(S)
    qb = jnp.moveaxis(q.reshape(B, H, nb, SB_QBLOCK, d), 2, 0)

    def one_block(args):
        q_blk, blk = args
        qpos = blk * SB_QBLOCK + jnp.arange(SB_QBLOCK)
        z = jnp.einsum('bhqd,bhkd->bhqk', q_blk, k, preferred_element_type=jnp.float32) * scale
        causal = kpos[None, :] < qpos[:, None]
        log_keep = jnp.where(causal, jax.nn.log_sigmoid(-z), 0.0)
        suffix = lax.cumsum(log_keep, axis=3, reverse=True) - log_keep
        a = jnp.where(causal, jnp.exp(jax.nn.log_sigmoid(z) + suffix), 0.0)
        return jnp.einsum('bhqk,bhkd->bhqd', a.astype(v.dtype), v)

    out = lax.map(one_block, (qb, jnp.arange(nb)))
    return jnp.moveaxis(out, 0, 2).reshape(B, H, S, d)


def sb_mixer(h, w_in, w_out):
    B, S, _ = h.shape
    qkv = (h @ w_in).reshape(B, S, 3, SB_HEADS, SB_HEAD_DIM)
    qkv = jnp.transpose(qkv, (2, 0, 3, 1, 4))
    o = stick_breaking_attention(qkv[0], qkv[1], qkv[2])
    return jnp.transpose(o, (0, 2, 1, 3)).reshape(B, S, SB_HEADS * SB_HEAD_DIM) @ w_out


def gla_chunked(q, k, v, log_a):
    B, H, S, dk = q.shape
    dv = v.shape[-1]
    C = GLA_CHUNK
    nc = S // C
    f32 = jnp.float32

    def chunks(a):
        return a.astype(f32).reshape(B, H, nc, C, a.shape[-1])

    q, k, v, g = chunks(q), chunks(k), chunks(v), chunks(log_a)
    b = jnp.cumsum(g, axis=3)
    b_last = b[:, :, :, -1:, :]
    q_in = q * jnp.exp(b)
    k_in = k * jnp.exp(-b)
    k_state = k * jnp.exp(b_last - b)
    causal = jnp.tril(jnp.ones((C, C), dtype=bool))
    scores = jnp.where(causal, jnp.einsum('bhnid,bhnjd->bhnij', q_in, k_in), 0.0)
    o_intra = jnp.einsum('bhnij,bhnjv->bhniv', scores, v)

    def step(state, inp):
        q_n, k_n, v_n, decay_n = inp
        o_n = jnp.einsum('bhid,bhdv->bhiv', q_n, state)
        state = decay_n[..., None] * state + jnp.einsum('bhjd,bhjv->bhdv', k_n, v_n)
        return state, o_n

    xs = (jnp.moveaxis(q_in, 2, 0), jnp.moveaxis(k_state, 2, 0), jnp.moveaxis(v, 2, 0),
          jnp.moveaxis(jnp.exp(b_last[:, :, :, 0, :]), 2, 0))
    _, o_inter = lax.scan(step, jnp.zeros((B, H, dk, dv), f32), xs)
    o = o_intra + jnp.moveaxis(o_inter, 0, 2)
    return o.reshape(B, H, S, dv)


def gla_mixer(h, w_in, w_gate_up, b_gate, norm_g, w_out):
    B, S, _ = h.shape
    dqk = GLA_HEADS * GLA_DK
    dv = GLA_HEADS * GLA_DV
    proj = h @ w_in
    q, k, v, r, g_down = jnp.split(proj, [dqk, 2 * dqk, 2 * dqk + dv, 2 * dqk + 2 * dv], axis=-1)
    log_a = jax.nn.log_sigmoid((g_down @ w_gate_up + b_gate).astype(jnp.float32)) / GLA_GATE_TAU

    def heads(a, d):
        return jnp.transpose(a.reshape(B, S, GLA_HEADS, d), (0, 2, 1, 3))

    o = gla_chunked(heads(q, GLA_DK) * GLA_DK ** -0.5, heads(k, GLA_DK),
                    heads(v, GLA_DV), heads(log_a, GLA_DK))
    o = rms_norm(o, norm_g)
    o = jnp.transpose(o, (0, 2, 1, 3)).reshape(B, S, dv).astype(h.dtype) * jax.nn.silu(r)
    return o @ w_out


def conv_mixer(h, w_in, conv_w, conv_b, w_out):
    S = h.shape[1]
    gate_b, gate_c, u = jnp.split(h @ w_in, 3, axis=-1)
    u = gate_c * u
    u_pad = jnp.pad(u, ((0, 0), (CONV_WIDTH - 1, 0), (0, 0)))
    y = sum(conv_w[j] * u_pad[:, j:j + S] for j in range(CONV_WIDTH)) + conv_b
    return (gate_b * y) @ w_out


def dilated_window_attention(q, k, v, window, dilation):
    B, H, S, d = q.shape
    Q = DIL_QBLOCK
    steps = window // dilation
    span = dilation * Q
    Sp = -(-S // span) * span
    L = Sp // dilation
    nb = L // Q

    def strided(a):
        a = jnp.pad(a, ((0, 0), (0, 0), (0, Sp - S), (0, 0)))
        a = jnp.transpose(a.reshape(B, H, L, dilation, d), (0, 1, 3, 2, 4))
        return a.reshape(B, H, dilation, nb, Q, d)

    def with_prev(a):
        prev = jnp.concatenate([jnp.zeros_like(a[:, :, :, :1]), a[:, :, :, :-1]], axis=3)
        return jnp.concatenate([prev, a], axis=4)

    qb = strided(q)
    kc, vc = with_prev(strided(k)), with_prev(strided(v))
    z = jnp.einsum('bhrnqd,bhrnkd->bhrnqk', qb, kc, preferred_element_type=jnp.float32) * d ** -0.5
    qi = jnp.arange(Q)[:, None]
    kj = jnp.arange(2 * Q)[None, :]
    dist = qi + Q - kj
    in_window = (dist >= 0) & (dist <= steps)
    before_start = (jnp.arange(nb)[:, None, None] == 0) & (kj < Q)[None]
    valid = in_window[None] & ~before_start
    z = jnp.where(valid, z, -jnp.inf)
    m = jnp.max(z, axis=-1, keepdims=True)
    lse = m + jnp.log(jnp.sum(jnp.exp(z - m), axis=-1, keepdims=True))
    p = jnp.exp(z - lse)
    o = jnp.einsum('bhrnqk,bhrnkd->bhrnqd', p.astype(vc.dtype), vc)

    def unstride(a):
        e = a.shape[-1]
        a = jnp.moveaxis(a.reshape(B, H, dilation, L, e), 2, 3).reshape(B, H, Sp, e)
        return a[:, :, :S]

    return unstride(o), unstride(lse)[..., 0]


def dil_mixer(h, w_in, w_out):
    B, S, _ = h.shape
    proj = (h @ w_in).reshape(B, S, DIL_GROUPS, 3, DIL_HEADS, DIL_HEAD_DIM)
    proj = jnp.transpose(proj, (2, 3, 0, 4, 1, 5))
    outs, lses = [], []
    for g, (window, dilation) in enumerate(DIL_PATTERNS):
        o_g, lse_g = dilated_window_attention(proj[g, 0], proj[g, 1], proj[g, 2], window, dilation)
        outs.append(o_g)
        lses.append(lse_g)
    o = jnp.stack(outs)
    w = jax.nn.softmax(jnp.stack(lses), axis=0)
    o = jnp.einsum('gbhs,gbhsd->bshd', w.astype(o.dtype), o).reshape(B, S, DIL_HEADS * DIL_HEAD_DIM)
    return o @ w_out


def hier_moe(h, w_grp, b_grp, w_exp, b_exp, w_gate, w_up, w_down):
    T, D = h.shape
    f32 = jnp.float32
    grp_logits = (h @ w_grp + b_grp).astype(f32)
    grp = jnp.argmax(grp_logits, axis=-1)
    p_grp = jnp.take_along_axis(jax.nn.softmax(grp_logits, axis=-1), grp[:, None], axis=-1)
    exp_logits = (h @ w_exp + b_exp).astype(f32).reshape(T, MOE_GROUPS, MOE_PER_GROUP)
    in_grp = jnp.take_along_axis(exp_logits, grp[:, None, None], axis=1)[:, 0]
    p_in, top_i = lax.top_k(jax.nn.softmax(in_grp, axis=-1), MOE_TOPK)
    gate = p_grp * p_in / jnp.sum(p_in, axis=-1, keepdims=True)
    expert = grp[:, None] * MOE_PER_GROUP + top_i

    A = T * MOE_TOPK
    flat_e = expert.reshape(A)
    order = jnp.argsort(flat_e)
    sorted_e = flat_e[order]
    sorted_tok = order // MOE_TOPK
    sorted_w = gate.reshape(A)[order]
    counts = jnp.bincount(flat_e, length=MOE_EXPERTS)
    padded = (counts + MOE_BLOCK - 1) // MOE_BLOCK * MOE_BLOCK
    pad_end = jnp.cumsum(padded)
    pad_start = pad_end - padded
    start = jnp.cumsum(counts) - counts
    dest = pad_start[sorted_e] + jnp.arange(A) - start[sorted_e]
    P = (-(-A // MOE_BLOCK) + MOE_EXPERTS) * MOE_BLOCK
    NB = P // MOE_BLOCK
    slot_tok = jnp.full((P,), T, dtype=jnp.int32).at[dest].set(sorted_tok.astype(jnp.int32))
    slot_w = jnp.zeros((P,), f32).at[dest].set(sorted_w)
    blk_e = jnp.minimum(jnp.searchsorted(pad_end, jnp.arange(NB) * MOE_BLOCK, side='right'),
                        MOE_EXPERTS - 1)
    h_pad = jnp.concatenate([h, jnp.zeros((1, D), h.dtype)], axis=0)
    xb = h_pad[slot_tok].reshape(NB, MOE_BLOCK, D)

    def expert_block(args):
        x_blk, e = args
        return (jax.nn.silu(x_blk @ w_gate[e]) * (x_blk @ w_up[e])) @ w_down[e]

    yb = lax.map(expert_block, (xb, blk_e)).reshape(P, D)
    y = jax.ops.segment_sum(yb.astype(f32) * slot_w[:, None], slot_tok, num_segments=T + 1)[:T]
    return y.astype(h.dtype)


def setup_inputs(seed: int = 0) -> dict:
    key = jax.random.key(seed)
    keys = jax.random.split(key, 26)
    D = D_MODEL

    def nrm(i, shape, scale):
        return jax.random.normal(keys[i], shape, jnp.float32) * scale

    sb_w = SB_HEADS * SB_HEAD_DIM
    gla_dqk = GLA_HEADS * GLA_DK
    gla_dv = GLA_HEADS * GLA_DV
    dil_w = DIL_HEADS * DIL_HEAD_DIM
    return {
        "x": nrm(0, (BATCH, SEQ, D), 1.0),
        "c": nrm(1, (BATCH, D), 1.0),
        "ada_w": nrm(2, (DEPTH, D, N_MOD * D), 0.5 * D ** -0.5),
        "ada_b": nrm(3, (DEPTH, N_MOD * D), 0.01),
        "norm_g": 1.0 + nrm(4, (DEPTH, 2, D), 0.02),
        "final_g": 1.0 + nrm(5, (D,), 0.02),
        "sb_w_in": nrm(6, (N_SB, D, 3 * sb_w), D ** -0.5),
        "sb_w_out": nrm(7, (N_SB, sb_w, D), sb_w ** -0.5),
        "gla_w_in": nrm(8, (N_GLA, D, GLA_IN), D ** -0.5),
        "gla_w_gate_up": nrm(9, (N_GLA, GLA_GATE_RANK, gla_dqk), GLA_GATE_RANK ** -0.5),
        "gla_b_gate": nrm(10, (N_GLA, gla_dqk), 0.01),
        "gla_norm_g": 1.0 + nrm(11, (N_GLA, GLA_DV), 0.02),
        "gla_w_out": nrm(12, (N_GLA, gla_dv, D), gla_dv ** -0.5),
        "conv_w_in": nrm(13, (N_CONV, D, 3 * D), D ** -0.5),
        "conv_w": nrm(14, (N_CONV, CONV_WIDTH, D), CONV_WIDTH ** -0.5),
        "conv_b": nrm(15, (N_CONV, D), 0.01),
        "conv_w_out": nrm(16, (N_CONV, D, D), D ** -0.5),
        "dil_w_in": nrm(17, (N_DIL, D, DIL_GROUPS * 3 * dil_w), D ** -0.5),
        "dil_w_out": nrm(18, (N_DIL, dil_w, D), dil_w ** -0.5),
        "moe_w_grp": nrm(19, (DEPTH, D, MOE_GROUPS), D ** -0.5),
        "moe_b_grp": nrm(20, (DEPTH, MOE_GROUPS), 0.01),
        "moe_w_exp": nrm(21, (DEPTH, D, MOE_EXPERTS), D ** -0.5),
        "moe_b_exp": nrm(22, (DEPTH, MOE_EXPERTS), 0.01),
        "moe_w_gate": nrm(23, (DEPTH, MOE_EXPERTS, D, MOE_D_FF), D ** -0.5),
        "moe_w_up": nrm(24, (DEPTH, MOE_EXPERTS, D, MOE_D_FF), D ** -0.5),
        "moe_w_down": nrm(25, (DEPTH, MOE_EXPERTS, MOE_D_FF, D), MOE_D_FF ** -0.5),
    }


def reference(x, c, ada_w, ada_b, norm_g, final_g, sb_w_in, sb_w_out, gla_w_in, gla_w_gate_up,
              gla_b_gate, gla_norm_g, gla_w_out, conv_w_in, conv_w, conv_b, conv_w_out,
              dil_w_in, dil_w_out, moe_w_grp, moe_b_grp, moe_w_exp, moe_b_exp, moe_w_gate,
              moe_w_up, moe_w_down):
    B, S, D = x.shape
    cond = jax.nn.silu(c)
    for i in range(DEPTH):
        mod = cond @ ada_w[i] + ada_b[i]
        sh1, sc1, g1, sh2, sc2, g2 = jnp.split(mod[:, None, :], N_MOD, axis=-1)

        h = rms_norm(x, norm_g[i, 0]) * (1.0 + sc1) + sh1
        kind, j = i % N_MIXERS, i // N_MIXERS
        if kind == 0:
            y = sb_mixer(h, sb_w_in[j], sb_w_out[j])
        elif kind == 1:
            y = gla_mixer(h, gla_w_in[j], gla_w_gate_up[j], gla_b_gate[j], gla_norm_g[j], gla_w_out[j])
        elif kind == 2:
            y = conv_mixer(h, conv_w_in[j], conv_w[j], conv_b[j], conv_w_out[j])
        else:
            y = dil_mixer(h, dil_w_in[j], dil_w_out[j])
        x = (x + g1 * y).astype(x.dtype)

        h = rms_norm(x, norm_g[i, 1]) * (1.0 + sc2) + sh2
        y = hier_moe(h.reshape(B * S, D), moe_w_grp[i], moe_b_grp[i], moe_w_exp[i], moe_b_exp[i],
                     moe_w_gate[i], moe_w_up[i], moe_w_down[i]).reshape(B, S, D)
        x = (x + g2 * y).astype(x.dtype)
    return rms_norm(x, final_g)
```

```python
import numpy as np
from contextlib import ExitStack
import concourse.bass as bass
import concourse.mybir as mybir
from concourse.bass_utils import run_bass_kernel_spmd

F32 = mybir.dt.float32
BF16 = mybir.dt.bfloat16
AF = mybir.ActivationFunctionType
ALU = mybir.AluOpType

D = 1024
T = 8192
KC = 8
DEPTH = 4
NG = T // 512
EPS = 1e-6
N_EXP = 32
DFF = 512

SP, ACT, DVE, POOL, PE = "sp", "act", "dve", "pool", "pe"
ALL_ENG = (SP, ACT, DVE, POOL, PE)
COMPUTE = (ACT, DVE, POOL, PE)


class Reg:
    __slots__ = ("w", "r", "multi")

    def __init__(self, multi=False):
        self.w = {}
        self.r = {}
        self.multi = multi


class Tl:
    def __init__(self, h, multi=False):
        self.h = h
        self.reg = Reg(multi)

    def __getitem__(self, idx):
        return self.h[idx]


class Sched:
    GEN = 30000
    NDMA = 8
    NATTACH = 1

    def __init__(self, nc, stack):
        self.nc = nc
        self.stack = stack
        self.eng = {SP: nc.sync, ACT: nc.scalar, DVE: nc.vector, POOL: nc.gpsimd, PE: nc.tensor}
        self.sems = []
        self.seq = {e: 0 for e in COMPUTE}
        self.esem = {e: [] for e in COMPUTE}
        self.own = {e: set() for e in ALL_ENG}
        self.waited = {e: {} for e in ALL_ENG}
        self.dsem = {}
        self.dnext = {}
        self.dval = {}
        self.n_ins = 0
        self.n_wait = 0
        for q in (SP, ACT, POOL):
            self.dsem[q] = [self._newsem("d%s%d" % (q, i)) for i in range(self.NDMA)]
            self.dnext[q] = 0
            for s in self.dsem[q]:
                self.dval[s] = 0

    def _newsem(self, name):
        h = self.stack.enter_context(self.nc.semaphore(name))
        self.sems.append(h)
        return len(self.sems) - 1

    def _deps(self, eng, reads, writes):
        deps = {}

        def add(s, v, raw):
            o = deps.get(s)
            if o is None:
                deps[s] = [v, raw]
            else:
                if v > o[0]:
                    o[0] = v
                o[1] = o[1] or raw

        for r in reads:
            for s, v in r.reg.w.items():
                add(s, v, True)
        for w in writes:
            for s, v in w.reg.r.items():
                add(s, v, False)
            if not w.reg.multi:
                for s, v in w.reg.w.items():
                    add(s, v, False)
        waits = []
        own = self.own[eng]
        wd = self.waited[eng]
        for s, (v, raw) in deps.items():
            if s in own:
                if eng == PE or not raw:
                    continue
            if wd.get(s, 0) >= v:
                continue
            wd[s] = v
            waits.append((s, v))
        return waits

    def _update(self, reads, writes, s, val):
        for r in reads:
            if r in writes:
                continue
            rr = r.reg.r
            if rr.get(s, 0) < val:
                rr[s] = val
        for w in writes:
            g = w.reg
            if g.r:
                g.w = {s: val}
                g.r = {}
            else:
                if g.w.get(s, 0) < val:
                    g.w[s] = val

    def _emit(self, eng, waits, fn, s, inc, attach_ok=False):
        e = self.eng[eng]
        attach = []
        if fn is not None and waits and attach_ok:
            attach = waits[-self.NATTACH:]
            waits = waits[:-self.NATTACH]
        for (ws, wv) in waits:
            e.wait_ge(self.sems[ws], wv)
            self.n_wait += 1
        if fn is not None:
            ins = fn(e)
            for (ws, wv) in attach:
                ins._wait_ge(self.sems[ws], wv)
            ins.then_inc(self.sems[s], inc)
            self.n_ins += 1

    def op(self, eng, fn, R=(), W=()):
        waits = self._deps(eng, R, W)
        self.seq[eng] += 1
        q = self.seq[eng]
        g = (q - 1) // self.GEN
        while len(self.esem[eng]) <= g:
            s_new = self._newsem("e%s%d" % (eng, len(self.esem[eng])))
            self.esem[eng].append(s_new)
            self.own[eng].add(s_new)
        s = self.esem[eng][g]
        val = q - g * self.GEN
        self._emit(eng, waits, fn, s, 1, attach_ok=True)
        self._update(R, W, s, val)

    def dma(self, eng, out, in_, R=(), W=()):
        waits = self._deps(eng, R, W)
        i = self.dnext[eng]
        self.dnext[eng] = (i + 1) % self.NDMA
        s = self.dsem[eng][i]
        prev = self.dval[s]
        wd = self.waited[eng]
        if prev > 0 and wd.get(s, 0) < prev:
            wd[s] = prev
            waits.append((s, prev))
        val = prev + 16
        self.dval[s] = val
        self._emit(eng, waits, lambda e: e.dma_start(out=out, in_=in_), s, 16)
        self._update(R, W, s, val)

    def _latest(self):
        lat = {}
        for e in COMPUTE:
            q = self.seq[e]
            if q > 0:
                g = (q - 1) // self.GEN
                lat[self.esem[e][g]] = q - g * self.GEN
        for s, v in self.dval.items():
            if v > 0:
                lat[s] = v
        return lat

    def barrier(self):
        lat = self._latest()
        for eng in ALL_ENG:
            wd = self.waited[eng]
            e = self.eng[eng]
            for s, v in lat.items():
                if eng in COMPUTE and s in self.own[eng]:
                    pass
                if wd.get(s, 0) >= v:
                    continue
                wd[s] = v
                e.wait_ge(self.sems[s], v)
                self.n_wait += 1

    def wait_all(self, eng):
        lat = self._latest()
        wd = self.waited[eng]
        e = self.eng[eng]
        for s, v in lat.items():
            if wd.get(s, 0) >= v:
                continue
            wd[s] = v
            e.wait_ge(self.sems[s], v)


class KB:
    def __init__(self, nc, stack):
        self.nc = nc
        self.gstack = stack
        self.s = Sched(nc, stack)
        self.stage_stack = None
        self._n = 0

    def name(self, p):
        self._n += 1
        return "%s_%d" % (p, self._n)

    def begin_stage(self):
        self.s.barrier()
        self.stage_stack = ExitStack()
        self.stage_stack.__enter__()

    def end_stage(self):
        self.s.barrier()
        self.stage_stack.__exit__(None, None, None)
        self.stage_stack = None

    def sb(self, shape, dtype, glob=False, multi=False):
        st = self.gstack if glob else self.stage_stack
        h = st.enter_context(self.nc.sbuf_tensor(self.name("sb"), list(shape), dtype))
        return Tl(h, multi)

    def ps(self, shape, dtype=F32, glob=False):
        st = self.gstack if glob else self.stage_stack
        h = st.enter_context(self.nc.psum_tensor(self.name("ps"), list(shape), dtype))
        return Tl(h)

    def dram(self, name, shape, dtype):
        h = self.nc.dram_tensor(name, list(shape), dtype)
        return Tl(h.ap(), multi=True)


def colview(ap2d):
    return ap2d.rearrange("(kc p) n -> p kc n", p=128)


def load_w_bf16(kb, dst, src_ap, R):
    kb.s.dma(POOL, dst_ap(dst), src_ap, R=R, W=[dst])


def dst_ap(t):
    return t.h[:]


def norm_group(kb, C, xg, a_col, sh_col, hT_bf, hT_f32=None):
    s = kb.s
    sq, ssp, rstd, tmp = C["n_sq"], C["n_ssp"], C["n_rstd"], C["n_tmp"]
    ones = C["ones_bf"]
    s.op(ACT, lambda e: e.activation(out=sq[:], in_=xg[:], func=AF.Square, scale=1.0 / 32.0),
         R=[xg], W=[sq])
    for kc in range(KC):
        s.op(PE, lambda e, kc=kc: e.matmul(ssp[:], lhsT=ones[:], rhs=sq[:, kc, :],
                                            start=(kc == 0), stop=(kc == KC - 1)),
             R=[ones, sq], W=[ssp])
    s.op(ACT, lambda e: e.activation(out=rstd[:], in_=ssp[:], func=AF.Sqrt, bias=C["eps_col"][:, 0:1]),
         R=[ssp, C["eps_col"]], W=[rstd])
    s.op(DVE, lambda e: e.reciprocal(out=rstd[:], in_=rstd[:]), R=[rstd], W=[rstd])
    at, ao = a_col
    st_, so = sh_col
    for kc in range(KC):
        s.op(DVE, lambda e, kc=kc: e.tensor_tensor(out=tmp[:, kc, :], in0=xg[:, kc, :], in1=rstd[:],
                                                    op=ALU.mult),
             R=[xg, rstd], W=[tmp])
    for kc in range(KC):
        if hT_f32 is not None:
            s.op(ACT, lambda e, kc=kc: e.activation(out=hT_f32[:, kc, :], in_=tmp[:, kc, :],
                                                     func=AF.Identity,
                                                     scale=at[:, ao + kc:ao + kc + 1],
                                                     bias=st_[:, so + kc:so + kc + 1]),
                 R=[tmp, at, st_], W=[hT_f32])
        else:
            s.op(ACT, lambda e, kc=kc: e.activation(out=hT_bf[:, kc, :], in_=tmp[:, kc, :],
                                                     func=AF.Identity,
                                                     scale=at[:, ao + kc:ao + kc + 1],
                                                     bias=st_[:, so + kc:so + kc + 1]),
                 R=[tmp, at, st_], W=[hT_bf])
    if hT_f32 is not None:
        s.op(POOL, lambda e: e.tensor_copy(out=hT_bf[:], in_=hT_f32[:]), R=[hT_f32], W=[hT_bf])


def alloc_norm_scratch(kb, C):
    C["n_sq"] = kb.sb([128, KC, 512], BF16)
    C["n_ssp"] = kb.ps([128, 512])
    C["n_rstd"] = kb.sb([128, 512], F32)
    C["n_tmp"] = kb.sb([128, KC, 512], F32)


def stage_consts(kb, C, IN):
    s = kb.s
    nc = kb.nc
    C["ones_bf"] = kb.sb([128, 128], BF16, glob=True)
    C["eps_col"] = kb.sb([128, 1], F32, glob=True)
    C["ident_f"] = kb.sb([128, 128], F32, glob=True)
    s.op(DVE, lambda e: e.memset(C["ones_bf"][:], 1.0), W=[C["ones_bf"]])
    s.op(DVE, lambda e: e.memset(C["eps_col"][:], EPS), W=[C["eps_col"]])
    s.dma(SP, C["ident_f"][:], IN["ident"][:, :], R=[IN["ident"]], W=[C["ident_f"]])
    C["mod"] = kb.sb([128, DEPTH * 48], F32, glob=True)
    C["a1"] = kb.sb([128, DEPTH * 8], F32, glob=True)
    C["a2"] = kb.sb([128, DEPTH * 8], F32, glob=True)
    C["normg"] = kb.sb([128, (DEPTH * 2 + 1) * 8], F32, glob=True)
    s.dma(SP, C["normg"][:], IN["normg_col"][:, :], R=[IN["normg_col"]], W=[C["normg"]])


def stage_mod(kb, C, IN):
    s = kb.s
    kb.begin_stage()
    ccol = kb.sb([128, 8], F32)
    cond = kb.sb([128, 8], F32)
    adab = kb.sb([128, DEPTH * 48], F32)
    s.dma(SP, ccol[:], IN["c_col"][:, :], R=[IN["c_col"]], W=[ccol])
    s.dma(SP, adab[:], IN["adab_col"][:, :], R=[IN["adab_col"]], W=[adab])
    s.op(ACT, lambda e: e.activation(out=cond[:], in_=ccol[:], func=AF.Silu), R=[ccol], W=[cond])
    wbuf = [kb.sb([128, KC, 1024], F32) for _ in range(2)]
    pss = [kb.ps([128, 8]) for _ in range(2)]
    it = 0
    for l in range(DEPTH):
        for j6 in range(6):
            wb = wbuf[it % 2]
            pst = pss[it % 2]
            it += 1
            src = IN["ada_w"][l, :, j6 * 1024:(j6 + 1) * 1024].rearrange("(kc p) n -> p kc n", p=128)
            s.dma(SP, wb[:], src, R=[IN["ada_w"]], W=[wb])
            for jn in range(8):
                for kc in range(KC):
                    s.op(PE, lambda e, wb=wb, pst=pst, jn=jn, kc=kc: e.matmul(
                        pst[:, jn:jn + 1], lhsT=wb[:, kc, jn * 128:(jn + 1) * 128],
                        rhs=cond[:, kc:kc + 1], start=(kc == 0), stop=(kc == KC - 1)),
                        R=[wb, cond], W=[pst])
            s.op(DVE, lambda e, pst=pst, l=l, j6=j6: e.tensor_tensor(
                out=C["mod"][:, l * 48 + j6 * 8:l * 48 + (j6 + 1) * 8], in0=pst[:],
                in1=adab[:, l * 48 + j6 * 8:l * 48 + (j6 + 1) * 8], op=ALU.add),
                R=[pst, adab], W=[C["mod"]])
    for l in range(DEPTH):
        for (dst, sub, off) in ((C["a1"], 0, 8), (C["a2"], 1, 32)):
            s.op(DVE, lambda e, dst=dst, sub=sub, off=off, l=l: e.scalar_tensor_tensor(
                out=dst[:, l * 8:(l + 1) * 8], in0=C["mod"][:, l * 48 + off:l * 48 + off + 8], scalar=1.0,
                in1=C["normg"][:, (l * 2 + sub) * 8:(l * 2 + sub + 1) * 8], op0=ALU.add, op1=ALU.mult),
                R=[C["mod"], C["normg"]], W=[dst])
    kb.end_stage()


def mod_col(C, l, which):
    off = {"sh1": 0, "sc1": 8, "g1": 16, "sh2": 24, "sc2": 32, "g2": 40}[which]
    return (C["mod"], l * 48 + off)


def flat_mod(C):
    return C["mod"]


def stage_outproj(kb, C, l, zT, KZ, w_out_ap, x_in, x_out):
    s = kb.s
    kb.begin_stage()
    w = kb.sb([128, KZ, D], BF16)
    s.dma(POOL, w[:], w_out_ap.rearrange("(kc p) n -> p kc n", p=128), R=[], W=[w])
    zb = [kb.sb([128, KZ, 512], BF16) for _ in range(2)]
    xb = [kb.sb([128, KC, 512], F32) for _ in range(2)]
    ob = [kb.sb([128, KC, 512], F32) for _ in range(2)]
    pss = [kb.ps([128, 512]) for _ in range(4)]
    modt = C["mod"]
    g1o = l * 48 + 16
    pi = 0
    for g in range(NG):
        z = zb[g % 2]
        xg = xb[g % 2]
        og = ob[g % 2]
        tsl = slice(g * 512, (g + 1) * 512)
        s.dma(SP, z[:], zT[0:KZ * 128, tsl].rearrange("(kc p) t -> p kc t", p=128), R=[zT], W=[z])
        s.dma(SP, xg[:], x_in[:, tsl].rearrange("(kc p) t -> p kc t", p=128), R=[x_in], W=[xg])
        for n in range(KC):
            pst = pss[pi % 4]
            pi += 1
            for kc in range(KZ):
                s.op(PE, lambda e, pst=pst, kc=kc, n=n, z=z: e.matmul(
                    pst[:], lhsT=w[:, kc, n * 128:(n + 1) * 128], rhs=z[:, kc, :],
                    start=(kc == 0), stop=(kc == KZ - 1)), R=[w, z], W=[pst])
            s.op(DVE, lambda e, pst=pst, n=n, xg=xg, og=og: e.scalar_tensor_tensor(
                out=og[:, n, :], in0=pst[:], scalar=modt[:, g1o + n:g1o + n + 1], in1=xg[:, n, :],
                op0=ALU.mult, op1=ALU.add), R=[pst, modt, xg], W=[og])
        s.dma(SP, x_out[:, tsl].rearrange("(kc p) t -> p kc t", p=128), og[:], R=[og], W=[x_out])
    kb.end_stage()


def stage_conv(kb, C, IN, l, x_in, zT):
    s = kb.s
    kb.begin_stage()
    alloc_norm_scratch(kb, C)
    w = kb.sb([128, KC, 3 * D], BF16)
    for j in range(3):
        s.dma(POOL, w[:, :, j * D:(j + 1) * D],
              IN["conv_w_in"][0, :, j * D:(j + 1) * D].rearrange("(kc p) n -> p kc n", p=128),
              R=[IN["conv_w_in"]], W=[w])
    cw = kb.sb([128, 24], F32)
    cb = kb.sb([128, 8], F32)
    s.dma(SP, cw[:], IN["conv_w_col"][:, :], R=[IN["conv_w_col"]], W=[cw])
    s.dma(SP, cb[:], IN["conv_b_col"][:, :], R=[IN["conv_b_col"]], W=[cb])
    xb = [kb.sb([128, KC, 512], F32) for _ in range(1)]
    hb = [kb.sb([128, KC, 512], BF16) for _ in range(1)]
    up = [kb.sb([128, KC, 514], F32) for _ in range(2)]
    yt = C["n_tmp"]
    zb = [kb.sb([128, KC, 512], BF16) for _ in range(2)]
    gcs = kb.sb([128, 512], F32)
    pss = [kb.ps([128, 512]) for _ in range(4)]
    a_col = (C["a1"], l * 8)
    sh_col = mod_col(C, l, "sh1")
    s.op(DVE, lambda e: e.memset(up[0][:, :, 0:2], 0.0), W=[up[0]])
    pi = 0
    for g in range(NG):
        xg = xb[0]
        hT = hb[0]
        u = up[g % 2]
        un = up[(g + 1) % 2]
        z = zb[g % 2]
        tsl = slice(g * 512, (g + 1) * 512)
        s.dma(SP, xg[:], x_in[:, tsl].rearrange("(kc p) t -> p kc t", p=128), R=[x_in], W=[xg])
        norm_group(kb, C, xg, a_col, sh_col, hT)

        def proj(n_off, n, pst):
            for kc in range(KC):
                s.op(PE, lambda e, kc=kc: e.matmul(
                    pst[:], lhsT=w[:, kc, n_off + n * 128:n_off + (n + 1) * 128], rhs=hT[:, kc, :],
                    start=(kc == 0), stop=(kc == KC - 1)), R=[w, hT], W=[pst])

        for n in range(KC):
            p1 = pss[pi % 4]; pi += 1
            proj(D, n, p1)
            s.op(ACT, lambda e, p1=p1: e.activation(out=gcs[:], in_=p1[:], func=AF.Identity),
                 R=[p1], W=[gcs])
            p2 = pss[pi % 4]; pi += 1
            proj(2 * D, n, p2)
            s.op(DVE, lambda e, p2=p2, n=n, u=u: e.tensor_tensor(
                out=u[:, n, 2:514], in0=p2[:], in1=gcs[:], op=ALU.mult), R=[p2, gcs], W=[u])
        if g + 1 < NG:
            s.op(POOL, lambda e, u=u, un=un: e.tensor_copy(out=un[:, :, 0:2], in_=u[:, :, 512:514]),
                 R=[u], W=[un])
        for n in range(KC):
            s.op(ACT, lambda e, n=n, u=u: e.activation(
                out=yt[:, n, :], in_=u[:, n, 2:514], func=AF.Identity,
                scale=cw[:, 16 + n:16 + n + 1], bias=cb[:, n:n + 1]), R=[u, cw, cb], W=[yt])
            s.op(DVE, lambda e, n=n, u=u: e.scalar_tensor_tensor(
                out=yt[:, n, :], in0=u[:, n, 1:513], scalar=cw[:, 8 + n:8 + n + 1], in1=yt[:, n, :],
                op0=ALU.mult, op1=ALU.add), R=[u, cw, yt], W=[yt])
            s.op(DVE, lambda e, n=n, u=u: e.scalar_tensor_tensor(
                out=yt[:, n, :], in0=u[:, n, 0:512], scalar=cw[:, n:n + 1], in1=yt[:, n, :],
                op0=ALU.mult, op1=ALU.add), R=[u, cw, yt], W=[yt])
            p3 = pss[pi % 4]; pi += 1
            proj(0, n, p3)
            s.op(DVE, lambda e, p3=p3, n=n, z=z: e.tensor_tensor(
                out=z[:, n, :], in0=p3[:], in1=yt[:, n, :], op=ALU.mult), R=[p3, yt], W=[z])
        s.dma(SP, zT[:, tsl].rearrange("(kc p) t -> p kc t", p=128), z[:], R=[z], W=[zT])
    kb.end_stage()


def stage_final(kb, C, x_in, out):
    s = kb.s
    kb.begin_stage()
    alloc_norm_scratch(kb, C)
    xb = [kb.sb([128, KC, 512], F32) for _ in range(2)]
    ob = [kb.sb([128, KC, 512], F32) for _ in range(2)]
    sq, ssp, rstd = C["n_sq"], C["n_ssp"], C["n_rstd"]
    ones = C["ones_bf"]
    fg = C["normg"]
    for g in range(NG):
        xg = xb[g % 2]
        og = ob[g % 2]
        tsl = slice(g * 512, (g + 1) * 512)
        s.dma(SP, xg[:], x_in[:, tsl].rearrange("(kc p) t -> p kc t", p=128), R=[x_in], W=[xg])
        s.op(ACT, lambda e, xg=xg: e.activation(out=sq[:], in_=xg[:], func=AF.Square, scale=1.0 / 32.0),
             R=[xg], W=[sq])
        for kc in range(KC):
            s.op(PE, lambda e, kc=kc: e.matmul(ssp[:], lhsT=ones[:], rhs=sq[:, kc, :],
                                                start=(kc == 0), stop=(kc == KC - 1)),
                 R=[ones, sq], W=[ssp])
        s.op(ACT, lambda e: e.activation(out=rstd[:], in_=ssp[:], func=AF.Sqrt, bias=C["eps_col"][:, 0:1]),
             R=[ssp, C["eps_col"]], W=[rstd])
        s.op(DVE, lambda e: e.reciprocal(out=rstd[:], in_=rstd[:]), R=[rstd], W=[rstd])
        for kc in range(KC):
            s.op(DVE, lambda e, kc=kc, xg=xg, og=og: e.scalar_tensor_tensor(
                out=og[:, kc, :], in0=xg[:, kc, :], scalar=fg[:, 2 * DEPTH * 8 + kc:2 * DEPTH * 8 + kc + 1], in1=rstd[:],
                op0=ALU.mult, op1=ALU.mult), R=[xg, fg, rstd], W=[og])
        s.dma(SP, out[:, tsl].rearrange("(kc p) t -> p kc t", p=128), og[:], R=[og], W=[out])
    kb.end_stage()


def stage_inproj(kb, C, l, x_in, w_ap, ncols, fm_specs, tm_specs):
    s = kb.s
    kb.begin_stage()
    alloc_norm_scratch(kb, C)
    w = kb.sb([128, KC, ncols], BF16)
    for c0 in range(0, ncols, 1024):
        c1 = min(ncols, c0 + 1024)
        s.dma(POOL, w[:, :, c0:c1], w_ap[:, c0:c1].rearrange("(kc p) n -> p kc n", p=128), R=[], W=[w])
    xg = kb.sb([128, KC, 512], F32)
    hT = kb.sb([128, KC, 512], BF16)
    fo = [kb.sb([128, 512], BF16) for _ in range(3)]
    to = [kb.sb([128, 512], BF16) for _ in range(3)]
    pss = [kb.ps([128, 512]) for _ in range(4)]
    a_col = (C["a1"], l * 8)
    sh_col = mod_col(C, l, "sh1")
    pi = 0
    fi = 0
    ti = 0
    for g in range(NG):
        tsl = slice(g * 512, (g + 1) * 512)
        s.dma(SP, xg[:], x_in[:, tsl].rearrange("(kc p) t -> p kc t", p=128), R=[x_in], W=[xg])
        norm_group(kb, C, xg, a_col, sh_col, hT)
        for (coff, nch, dst, roff, scale) in fm_specs:
            for n in range(nch):
                pst = pss[pi % 4]; pi += 1
                o = fo[fi % 3]; fi += 1
                for kc in range(KC):
                    s.op(PE, lambda e, kc=kc: e.matmul(pst[:], lhsT=w[:, kc, coff + n * 128:coff + (n + 1) * 128],
                                                        rhs=hT[:, kc, :], start=(kc == 0), stop=(kc == KC - 1)),
                         R=[w, hT], W=[pst])
                if (pi % 2) == 0:
                    s.op(ACT, lambda e: e.activation(out=o[:], in_=pst[:], func=AF.Identity, scale=float(scale)),
                         R=[pst], W=[o])
                else:
                    s.op(DVE, lambda e: e.tensor_scalar(out=o[:], in0=pst[:], scalar1=float(scale), scalar2=None,
                                                        op0=ALU.mult), R=[pst], W=[o])
                s.dma(SP, dst[roff + n * 128:roff + (n + 1) * 128, tsl], o[:], R=[o], W=[dst])
        for (coff, ncl, dst, doff) in tm_specs:
            for j in range(4):
                for c in range(ncl // 512):
                    pst = pss[pi % 4]; pi += 1
                    o = to[ti % 3]; ti += 1
                    for kc in range(KC):
                        s.op(PE, lambda e, kc=kc: e.matmul(pst[:], lhsT=hT[:, kc, j * 128:(j + 1) * 128],
                                                            rhs=w[:, kc, coff + c * 512:coff + (c + 1) * 512],
                                                            start=(kc == 0), stop=(kc == KC - 1)),
                             R=[w, hT], W=[pst])
                    if (pi % 2) == 0:
                        s.op(ACT, lambda e: e.activation(out=o[:], in_=pst[:], func=AF.Identity), R=[pst], W=[o])
                    else:
                        s.op(DVE, lambda e: e.tensor_copy(out=o[:], in_=pst[:]), R=[pst], W=[o])
                    s.dma(SP, dst[g * 512 + j * 128:g * 512 + (j + 1) * 128, doff + c * 512:doff + (c + 1) * 512],
                          o[:], R=[o], W=[dst])
    kb.end_stage()


def stage_sb_attn(kb, C, IN, qT_d, kT_d, v_d, oT_d, n_heads=16):
    s = kb.s
    kb.begin_stage()
    ones = C["ones_bf"]
    ntri = kb.sb([128, 128], BF16)
    s.dma(POOL, ntri[:], IN["ntri"][:, :], R=[IN["ntri"]], W=[ntri])
    masks = kb.sb([128, 4, 512], BF16)
    s.dma(POOL, masks[:], IN["sbmask"][:, :, :], R=[IN["sbmask"]], W=[masks])
    qh = [kb.sb([128, T], BF16) for _ in range(2)]
    kh = [kb.sb([128, T], BF16) for _ in range(2)]
    vh = [kb.sb([128, 64, 128], BF16) for _ in range(2)]
    for b in range(2):
        s.op(POOL, lambda e, b=b: e.memset(qh[b][:], 0.0), W=[qh[b]])
        s.op(POOL, lambda e, b=b: e.memset(kh[b][:], 0.0), W=[kh[b]])
        s.op(POOL, lambda e, b=b: e.memset(vh[b][:], 0.0), W=[vh[b]])
    NB = 3
    bank = [kb.ps([128, 512]) for _ in range(NB)]
    Op = [kb.ps([128, 512]) for _ in range(2)]
    oacc = [kb.ps([128, 512]) for _ in range(2)]
    eb = [kb.sb([128, 512], F32) for _ in range(2)]
    spb = [kb.sb([128, 512], BF16) for _ in range(3)]
    t3b = [kb.sb([128, 512], F32) for _ in range(2)]
    ab = [kb.sb([128, 512], BF16) for _ in range(3)]
    carry = [kb.sb([128, 512], F32) for _ in range(2)]
    otb = [kb.sb([64, 512], BF16) for _ in range(2)]

    tiles = []
    for h in range(n_heads):
        for g in range(NG):
            nk = 4 * g + 4
            for ik in range(nk):
                kbk = nk - 1 - ik
                tiles.append((h, g, kbk, ik == 0, kbk == 0, (kbk - 4 * g) if kbk >= 4 * g else -1))

    loaded = set()

    def ensure_head(h):
        if h in loaded or h >= n_heads:
            return
        loaded.add(h)
        b = h % 2
        s.dma(SP, qh[b][0:64, :], qT_d[h * 64:(h + 1) * 64, :], R=[qT_d], W=[qh[b]])
        s.dma(SP, kh[b][0:64, :], kT_d[h * 64:(h + 1) * 64, :], R=[kT_d], W=[kh[b]])
        for c in range(4):
            s.dma(SP, vh[b][:, c * 16:(c + 1) * 16, 0:64],
                  v_d[c * 2048:(c + 1) * 2048, h * 64:(h + 1) * 64].rearrange("(kb p) d -> p kb d", p=128),
                  R=[v_d], W=[vh[b]])

    def stA(i):
        h, g, kbk, first, last, dg = tiles[i]
        ensure_head(h)
        if first and g == 0:
            ensure_head(h + 1)
        b = h % 2
        bk = bank[i % NB]
        e_ = eb[i % 2]
        sp = spb[i % 3]
        s.op(PE, lambda e: e.matmul(bk[:], lhsT=kh[b][:, kbk * 128:(kbk + 1) * 128], rhs=qh[b][:, g * 512:(g + 1) * 512],
                                    start=True, stop=False), R=[kh[b], qh[b]], W=[bk])
        s.op(ACT, lambda e: e.activation(out=e_[:], in_=bk[:], func=AF.Exp), R=[bk], W=[e_])

    def stA2(i):
        h, g, kbk, first, last, dg = tiles[i]
        e_ = eb[i % 2]
        sp = spb[i % 3]
        s.op(ACT, lambda e: e.activation(out=sp[:], in_=e_[:], func=AF.Ln, bias=1.0), R=[e_], W=[sp])
        if dg >= 0:
            s.op(DVE, lambda e: e.tensor_tensor(out=sp[:], in0=sp[:], in1=masks[:, dg, :], op=ALU.mult),
                 R=[sp, masks], W=[sp])

    def stB(i):
        h, g, kbk, first, last, dg = tiles[i]
        bk = bank[i % NB]
        sp = spb[i % 3]
        o_ = Op[i % 2]
        a_ = ab[i % 3]
        t3 = t3b[i % 2]
        cr = carry[(h * NG + g) % 2]
        s.op(PE, lambda e: e.matmul(bk[:], lhsT=ntri[:], rhs=sp[:], start=False, stop=True), R=[ntri, sp], W=[bk])
        if not last:
            s.op(PE, lambda e: e.matmul(o_[:], lhsT=ones[:], rhs=sp[:], start=True, stop=True), R=[ones, sp], W=[o_])
        if first:
            s.op(ACT, lambda e: e.activation(out=a_[:], in_=bk[:], func=AF.Exp), R=[bk], W=[a_])
            if not last:
                s.op(DVE, lambda e: e.tensor_copy(out=cr[:], in_=o_[:]), R=[o_], W=[cr])
        else:
            s.op(DVE, lambda e: e.tensor_tensor(out=t3[:], in0=bk[:], in1=cr[:], op=ALU.subtract), R=[bk, cr], W=[t3])
            s.op(ACT, lambda e: e.activation(out=a_[:], in_=t3[:], func=AF.Exp), R=[t3], W=[a_])
            if not last:
                s.op(DVE, lambda e: e.tensor_tensor(out=cr[:], in0=o_[:], in1=cr[:], op=ALU.add), R=[o_, cr], W=[cr])
        if dg >= 0:
            s.op(DVE, lambda e: e.tensor_tensor(out=a_[:], in0=a_[:], in1=masks[:, dg, :], op=ALU.mult),
                 R=[a_, masks], W=[a_])

    def stC(i):
        h, g, kbk, first, last, dg = tiles[i]
        b = h % 2
        a_ = ab[i % 3]
        oa = oacc[(h * NG + g) % 2]
        s.op(PE, lambda e: e.matmul(oa[:], lhsT=vh[b][:, kbk, :], rhs=a_[:], start=first, stop=last),
             R=[vh[b], a_], W=[oa])
        if last:
            ot = otb[(h * NG + g) % 2]
            s.op(DVE, lambda e: e.tensor_copy(out=ot[:], in_=oa[0:64, :]), R=[oa], W=[ot])
            s.dma(SP, oT_d[h * 64:(h + 1) * 64, g * 512:(g + 1) * 512], ot[:], R=[ot], W=[oT_d])

    n = len(tiles)
    for i in range(n + 2):
        if i < n:
            stA(i)
        if 0 <= i - 1 < n:
            stB(i - 1)
        if i < n:
            stA2(i)
        if 0 <= i - 2 < n:
            stC(i - 2)
    kb.end_stage()


DIL_PAT = ((128, 1), (512, 4), (2048, 16))
DIL_MOFF = (0, 5, 13)
DIL_NMASK = 33


def stage_dil_attn(kb, C, IN, dq_d, dk_d, dv_d, oT_d, n_heads=8):
    s = kb.s
    kb.begin_stage()
    masks = kb.sb([128, DIL_NMASK, 512], BF16)
    for c0 in range(0, DIL_NMASK, 11):
        s.dma(POOL, masks[:, c0:c0 + 11, :], IN["dilmask"][:, c0:c0 + 11, :], R=[IN["dilmask"]], W=[masks])
    shift = kb.sb([128, 64], F32)
    s.dma(SP, shift[:], IN["shift"][:, :], R=[IN["shift"]], W=[shift])
    qh = [kb.sb([128, T], BF16) for _ in range(2)]
    kh = [kb.sb([128, T], BF16) for _ in range(2)]
    vh = [kb.sb([128, 64, 128], BF16) for _ in range(2)]
    for b in range(2):
        s.op(POOL, lambda e, b=b: e.memset(qh[b][:], 0.0), W=[qh[b]])
        s.op(POOL, lambda e, b=b: e.memset(kh[b][:], 0.0), W=[kh[b]])
        s.op(POOL, lambda e, b=b: e.memset(vh[b][:], 1.0), W=[vh[b]])
    acc = kb.sb([128, NG, 512], F32)
    zp = [kb.ps([128, 512]) for _ in range(3)]
    ndp = [kb.ps([128, 512]) for _ in range(2)]
    dnp = [kb.ps([64, 512]) for _ in range(2)]
    pb = [kb.sb([128, 512], BF16) for _ in range(3)]
    rdb = [kb.sb([64, 512], F32) for _ in range(2)]
    otb = [kb.sb([64, 512], BF16) for _ in range(2)]

    units = [(hd, dg) for hd in range(n_heads) for dg in range(3)]
    tiles = []
    for u, (hd, dg) in enumerate(units):
        W_, r_ = DIL_PAT[dg]
        wb = W_ // 128
        for g in range(NG):
            lo = max(0, 4 * g - wb)
            hi = 4 * g + 3
            for kbk in range(lo, hi + 1):
                rel = kbk - (4 * g - wb)
                tiles.append((u, hd, dg, g, kbk, kbk == lo, kbk == hi, DIL_MOFF[dg] + rel))

    loaded = set()

    def ensure_unit(u):
        if u in loaded or u >= len(units):
            return
        loaded.add(u)
        hd, dg = units[u]
        b = u % 2
        r0 = dg * 512 + hd * 64
        s.dma(SP, qh[b][0:64, :], dq_d[r0:r0 + 64, :], R=[dq_d], W=[qh[b]])
        s.dma(SP, kh[b][0:64, :], dk_d[r0:r0 + 64, :], R=[dk_d], W=[kh[b]])
        for c in range(4):
            s.dma(SP, vh[b][:, c * 16:(c + 1) * 16, 0:64],
                  dv_d[c * 2048:(c + 1) * 2048, r0:r0 + 64].rearrange("(kb p) d -> p kb d", p=128),
                  R=[dv_d], W=[vh[b]])

    def stA(i):
        u, hd, dg, g, kbk, first, last, mi = tiles[i]
        ensure_unit(u)
        if first and g == 0:
            ensure_unit(u + 1)
        b = u % 2
        z = zp[i % 3]
        p = pb[i % 3]
        s.op(PE, lambda e: e.matmul(z[:], lhsT=kh[b][:, kbk * 128:(kbk + 1) * 128], rhs=qh[b][:, g * 512:(g + 1) * 512],
                                    start=True, stop=True), R=[kh[b], qh[b]], W=[z])
        s.op(ACT, lambda e: e.activation(out=p[:], in_=z[:], func=AF.Exp), R=[z], W=[p])
        s.op(DVE, lambda e: e.tensor_tensor(out=p[:], in0=p[:], in1=masks[:, mi, :], op=ALU.mult),
             R=[p, masks], W=[p])

    def stB(i):
        u, hd, dg, g, kbk, first, last, mi = tiles[i]
        b = u % 2
        p = pb[i % 3]
        nd = ndp[(u * NG + g) % 2]
        s.op(PE, lambda e: e.matmul(nd[:], lhsT=vh[b][:, kbk, :], rhs=p[:], start=first, stop=last),
             R=[vh[b], p], W=[nd])
        if last:
            if dg == 0:
                s.op(DVE, lambda e: e.tensor_copy(out=acc[:, g, :], in_=nd[:]), R=[nd], W=[acc])
            else:
                s.op(DVE, lambda e: e.tensor_tensor(out=acc[:, g, :], in0=nd[:], in1=acc[:, g, :], op=ALU.add),
                     R=[nd, acc], W=[acc])
            if dg == 2:
                dn = dnp[g % 2]
                rd = rdb[g % 2]
                ot = otb[g % 2]
                s.op(PE, lambda e: e.matmul(dn[:], lhsT=shift[:], rhs=acc[:, g, :], start=True, stop=True),
                     R=[shift, acc], W=[dn])
                s.op(DVE, lambda e: e.reciprocal(out=rd[:], in_=dn[:]), R=[dn], W=[rd])
                s.op(DVE, lambda e: e.tensor_tensor(out=ot[:], in0=acc[0:64, g, :], in1=rd[:], op=ALU.mult),
                     R=[acc, rd], W=[ot])
                s.dma(SP, oT_d[hd * 64:(hd + 1) * 64, g * 512:(g + 1) * 512], ot[:], R=[ot], W=[oT_d])

    n = len(tiles)
    for i in range(n + 1):
        if i < n:
            stA(i)
        if 0 <= i - 1 < n:
            stB(i - 1)
    kb.end_stage()


GDK = 128
GDV = 256


def stage_gla_proj(kb, C, IN, l, x_in, gq_d, gk_d, gks_d, gv_d, gsr_d, gdec_d):
    s = kb.s
    kb.begin_stage()
    alloc_norm_scratch(kb, C)
    NCOL = 3088
    w = kb.sb([128, KC, NCOL], BF16)
    w_ap = IN["gla_w_in"][0, :, :]
    for c0 in range(0, NCOL, 1024):
        c1 = min(NCOL, c0 + 1024)
        s.dma(POOL, w[:, :, c0:c1], w_ap[:, c0:c1].rearrange("(kc p) n -> p kc n", p=128), R=[], W=[w])
    wgu = kb.sb([32, 512], BF16)
    s.dma(POOL, wgu[0:17, :], IN["gla_wgu"][:, :], R=[IN["gla_wgu"]], W=[wgu])
    triblk = kb.sb([128, 128], BF16)
    upblk = kb.sb([128, 128], BF16)
    s.dma(POOL, triblk[:], IN["gla_tri"][:, :], R=[IN["gla_tri"]], W=[triblk])
    s.dma(POOL, upblk[:], IN["gla_up"][:, :], R=[IN["gla_up"]], W=[upblk])
    gd = kb.sb([32, 512], BF16)
    s.op(DVE, lambda e: e.memset(gd[:], 1.0), W=[gd])
    xg = kb.sb([128, KC, 512], F32)
    hT = kb.sb([128, KC, 512], BF16)
    sptok = [kb.sb([128, 512], BF16) for _ in range(4)]
    ef = [kb.sb([128, 512], F32) for _ in range(2)]
    ebf = [kb.sb([128, 512], F32) for _ in range(2)]
    enb = [kb.sb([128, 512], F32) for _ in range(2)]
    ob = [kb.sb([128, 512], BF16) for _ in range(4)]
    dcol = [kb.sb([128, 8], F32) for _ in range(2)]
    pss = [kb.ps([128, 512]) for _ in range(6)]
    a_col = (C["a1"], l * 8)
    sh_col = mod_col(C, l, "sh1")
    st = {"p": 0, "o": 0}

    def nps():
        st["p"] += 1
        return pss[st["p"] % 6]

    def nob():
        st["o"] += 1
        return ob[st["o"] % 4]

    def proj_fm(pst, coff, ncols=128):
        for kc in range(KC):
            s.op(PE, lambda e, kc=kc: e.matmul(pst[0:ncols, :], lhsT=w[:, kc, coff:coff + ncols], rhs=hT[:, kc, :],
                                                start=(kc == 0), stop=(kc == KC - 1)), R=[w, hT], W=[pst])

    def proj_tm(pst, j, coff):
        for kc in range(KC):
            s.op(PE, lambda e, kc=kc: e.matmul(pst[:], lhsT=hT[:, kc, j * 128:(j + 1) * 128],
                                                rhs=w[:, kc, coff:coff + 512],
                                                start=(kc == 0), stop=(kc == KC - 1)), R=[w, hT], W=[pst])

    for g in range(NG):
        tsl = slice(g * 512, (g + 1) * 512)
        s.dma(SP, xg[:], x_in[:, tsl].rearrange("(kc p) t -> p kc t", p=128), R=[x_in], W=[xg])
        norm_group(kb, C, xg, a_col, sh_col, hT)
        p = nps()
        proj_fm(p, 3072, 16)
        s.op(ACT, lambda e: e.activation(out=gd[0:16, :], in_=p[0:16, :], func=AF.Identity), R=[p], W=[gd])
        for j in range(4):
            p = nps()
            e_ = ef[j % 2]
            s.op(PE, lambda e: e.matmul(p[:], lhsT=gd[0:17, j * 128:(j + 1) * 128], rhs=wgu[0:17, :],
                                        start=True, stop=True), R=[gd, wgu], W=[p])
            s.op(ACT, lambda e: e.activation(out=e_[:], in_=p[:], func=AF.Exp, scale=-1.0), R=[p], W=[e_])
            s.op(ACT, lambda e: e.activation(out=sptok[j][:], in_=e_[:], func=AF.Ln, bias=1.0), R=[e_], W=[sptok[j]])
        for h in range(4):
            p = nps()
            for j in range(4):
                s.op(PE, lambda e, j=j: e.matmul(p[:, j * 128:(j + 1) * 128], lhsT=sptok[j][:, h * 128:(h + 1) * 128],
                                                  rhs=triblk[:], start=True, stop=True), R=[sptok[j], triblk], W=[p])
            eb_, en_ = ebf[h % 2], enb[h % 2]
            s.op(ACT, lambda e: e.activation(out=eb_[:], in_=p[:], func=AF.Exp), R=[p], W=[eb_])
            s.op(ACT, lambda e: e.activation(out=en_[:], in_=p[:], func=AF.Exp, scale=-1.0), R=[p], W=[en_])
            dc = dcol[h % 2]
            s.op(POOL, lambda e: e.tensor_copy(out=dc[:], in_=eb_[:].rearrange("p (c t) -> p c t", t=64)[:, :, 63]),
                 R=[eb_], W=[dc])
            s.dma(SP, gdec_d[:, h * 128 + g * 8:h * 128 + (g + 1) * 8], dc[:], R=[dc], W=[gdec_d])
            pq = nps()
            proj_fm(pq, h * 128)
            o = nob()
            s.op(DVE, lambda e: e.scalar_tensor_tensor(out=o[:], in0=pq[:], scalar=float(GDK ** -0.5), in1=eb_[:],
                                                       op0=ALU.mult, op1=ALU.mult), R=[pq, eb_], W=[o])
            s.dma(SP, gq_d[h * 128:(h + 1) * 128, tsl], o[:], R=[o], W=[gq_d])
            pk = nps()
            proj_fm(pk, 512 + h * 128)
            o2 = nob()
            s.op(DVE, lambda e: e.tensor_tensor(out=o2[:], in0=pk[:], in1=en_[:], op=ALU.mult), R=[pk, en_], W=[o2])
            s.dma(SP, gk_d[h * 128:(h + 1) * 128, tsl], o2[:], R=[o2], W=[gk_d])
        for j in range(4):
            pk = nps()
            proj_tm(pk, j, 512)
            pd = nps()
            s.op(PE, lambda e: e.matmul(pd[:], lhsT=upblk[:], rhs=sptok[j][:], start=True, stop=True),
                 R=[upblk, sptok[j]], W=[pd])
            e_ = ef[j % 2]
            s.op(ACT, lambda e: e.activation(out=e_[:], in_=pd[:], func=AF.Exp), R=[pd], W=[e_])
            o = nob()
            s.op(DVE, lambda e: e.tensor_tensor(out=o[:], in0=pk[:], in1=e_[:], op=ALU.mult), R=[pk, e_], W=[o])
            s.dma(SP, gks_d[g * 512 + j * 128:g * 512 + (j + 1) * 128, :], o[:], R=[o], W=[gks_d])
        for j in range(4):
            for c in range(2):
                pv = nps()
                proj_tm(pv, j, 1024 + c * 512)
                o = nob()
                s.op(ACT, lambda e: e.activation(out=o[:], in_=pv[:], func=AF.Identity), R=[pv], W=[o])
                s.dma(SP, gv_d[g * 512 + j * 128:g * 512 + (j + 1) * 128, c * 512:(c + 1) * 512], o[:], R=[o], W=[gv_d])
        for n in range(8):
            pr = nps()
            proj_fm(pr, 2048 + n * 128)
            o = nob()
            s.op(ACT, lambda e: e.activation(out=o[:], in_=pr[:], func=AF.Silu), R=[pr], W=[o])
            s.dma(SP, gsr_d[n * 128:(n + 1) * 128, tsl], o[:], R=[o], W=[gsr_d])
    kb.end_stage()


def stage_gla_scan(kb, C, IN, gq_d, gk_d, gks_d, gv_d, gsr_d, gdec_d, zT_d):
    s = kb.s
    kb.begin_stage()
    ones = C["ones_bf"]
    cmask = kb.sb([128, 128], BF16)
    s.dma(POOL, cmask[:], IN["gla_cmask"][:, :], R=[IN["gla_cmask"]], W=[cmask])
    ng = kb.sb([128, 2], F32)
    s.dma(SP, ng[:], IN["gla_ng_col"][:, :], R=[IN["gla_ng_col"]], W=[ng])
    dec = kb.sb([128, 512], F32)
    s.dma(SP, dec[:], gdec_d[:, :], R=[gdec_d], W=[dec])
    qin = [kb.sb([128, 4, 512], BF16) for _ in range(2)]
    kin = [kb.sb([128, 4, 512], BF16) for _ in range(2)]
    ksb = [kb.sb([128, 4, 512], BF16) for _ in range(2)]
    vb = [kb.sb([128, 4, 1024], BF16) for _ in range(2)]
    srb = [kb.sb([128, 8, 512], BF16) for _ in range(2)]
    ztb = [kb.sb([128, 8, 512], BF16) for _ in range(2)]
    state = [kb.sb([128, GDV], F32) for _ in range(4)]
    sbf = [[kb.sb([128, GDV], BF16) for _ in range(3)] for _ in range(4)]
    for h in range(4):
        s.op(DVE, lambda e, h=h: e.memset(state[h][:], 0.0), W=[state[h]])
        s.op(DVE, lambda e, h=h: e.memset(sbf[h][0][:], 0.0), W=[sbf[h][0]])
    scp = [kb.ps([128, 128]) for _ in range(2)]
    upp = [kb.ps([128, GDV]) for _ in range(2)]
    otp = [kb.ps([128, 128]) for _ in range(2)]
    ssp = [kb.ps([128, 128]) for _ in range(2)]
    scm = [kb.sb([128, 128], BF16) for _ in range(2)]
    sqb = [kb.sb([128, 128], BF16) for _ in range(4)]
    rsb = [kb.sb([128, 128], F32) for _ in range(2)]
    tb = [kb.sb([128, 128], F32) for _ in range(2)]
    ver = [0, 0, 0, 0]
    cnt = {"u": 0, "o": 0, "q": 0, "x": 0}
    for G in range(NG):
        b = G % 2
        tsl = slice(G * 512, (G + 1) * 512)
        s.dma(SP, qin[b][:], gq_d[:, tsl].rearrange("(h p) t -> p h t", p=128), R=[gq_d], W=[qin[b]])
        s.dma(SP, kin[b][:], gk_d[:, tsl].rearrange("(h p) t -> p h t", p=128), R=[gk_d], W=[kin[b]])
        s.dma(SP, ksb[b][:], gks_d[G * 512:(G + 1) * 512, :].rearrange("(j p) c -> p j c", p=128), R=[gks_d], W=[ksb[b]])
        s.dma(SP, vb[b][:], gv_d[G * 512:(G + 1) * 512, :].rearrange("(j p) c -> p j c", p=128), R=[gv_d], W=[vb[b]])
        s.dma(SP, srb[b][:], gsr_d[:, tsl].rearrange("(n p) t -> p n t", p=128), R=[gsr_d], W=[srb[b]])
        Q, Kk, KS, V, SR, ZT = qin[b], kin[b], ksb[b], vb[b], srb[b], ztb[b]
        for j in range(4):
            c0 = 2 * (4 * G + j)
            js = slice(j * 128, (j + 1) * 128)
            for h in range(4):
                cnt["x"] += 1
                x_ = cnt["x"]
                sc = scp[x_ % 2]
                sm = scm[x_ % 2]
                s.op(PE, lambda e: e.matmul(sc[:], lhsT=Kk[:, h, js], rhs=Q[:, h, js], start=True, stop=True),
                     R=[Kk, Q], W=[sc])
                s.op(DVE, lambda e: e.tensor_tensor(out=sm[:], in0=sc[:], in1=cmask[:], op=ALU.mult),
                     R=[sc, cmask], W=[sm])
                S0 = sbf[h][ver[h] % 3]
                S1 = sbf[h][(ver[h] + 1) % 3]
                S2 = sbf[h][(ver[h] + 2) % 3]
                ver[h] += 2
                for ci, (lo, Snew) in enumerate(((0, S1), (64, S2))):
                    cnt["u"] += 1
                    up = upp[cnt["u"] % 2]
                    s.op(PE, lambda e, lo=lo: e.matmul(up[:], lhsT=KS[lo:lo + 64, j, h * 128:(h + 1) * 128],
                                                        rhs=V[lo:lo + 64, j, h * 256:(h + 1) * 256],
                                                        start=True, stop=True), R=[KS, V], W=[up])
                    cc = h * 128 + c0 + ci
                    s.op(DVE, lambda e, cc=cc: e.scalar_tensor_tensor(
                        out=state[h][:], in0=state[h][:], scalar=dec[:, cc:cc + 1], in1=up[:],
                        op0=ALU.mult, op1=ALU.add), R=[state[h], dec, up], W=[state[h]])
                    s.op(ACT, lambda e, Snew=Snew: e.activation(out=Snew[:], in_=state[h][:], func=AF.Identity),
                         R=[state[h]], W=[Snew])
                ots = []
                cnt["q"] += 1
                ss = ssp[cnt["q"] % 2]
                for half in range(2):
                    cnt["o"] += 1
                    ot = otp[cnt["o"] % 2]
                    vs = slice(h * 256 + half * 128, h * 256 + (half + 1) * 128)
                    hs = slice(half * 128, (half + 1) * 128)
                    s.op(PE, lambda e: e.matmul(ot[:], lhsT=V[:, j, vs], rhs=sm[:], start=True, stop=False),
                         R=[V, sm], W=[ot])
                    s.op(PE, lambda e: e.matmul(ot[:, 0:64], lhsT=S0[:, hs], rhs=Q[:, h, j * 128:j * 128 + 64],
                                                start=False, stop=False), R=[S0, Q], W=[ot])
                    s.op(PE, lambda e: e.matmul(ot[:, 64:128], lhsT=S1[:, hs], rhs=Q[:, h, j * 128 + 64:(j + 1) * 128],
                                                start=False, stop=True), R=[S1, Q], W=[ot])
                    sq = sqb[(cnt["o"]) % 4]
                    s.op(ACT, lambda e: e.activation(out=sq[:], in_=ot[:], func=AF.Square, scale=1.0 / 16.0),
                         R=[ot], W=[sq])
                    s.op(PE, lambda e, half=half: e.matmul(ss[:], lhsT=ones[:], rhs=sq[:], start=(half == 0),
                                                            stop=(half == 1)), R=[ones, sq], W=[ss])
                    ots.append(ot)
                rs = rsb[cnt["q"] % 2]
                s.op(ACT, lambda e: e.activation(out=rs[:], in_=ss[:], func=AF.Sqrt, bias=C["eps_col"][:, 0:1]),
                     R=[ss, C["eps_col"]], W=[rs])
                s.op(DVE, lambda e: e.reciprocal(out=rs[:], in_=rs[:]), R=[rs], W=[rs])
                for half in range(2):
                    ot = ots[half]
                    t_ = tb[half]
                    s.op(DVE, lambda e: e.tensor_tensor(out=t_[:], in0=ot[:], in1=rs[:], op=ALU.mult),
                         R=[ot, rs], W=[t_])
                    s.op(DVE, lambda e, half=half: e.scalar_tensor_tensor(
                        out=ZT[:, h * 2 + half, js], in0=t_[:], scalar=ng[:, half:half + 1],
                        in1=SR[:, h * 2 + half, js], op0=ALU.mult, op1=ALU.mult), R=[t_, ng, SR], W=[ZT])
        s.dma(SP, zT_d[:, tsl].rearrange("(n p) t -> p n t", p=128), ZT[:], R=[ZT], W=[zT_d])
    kb.end_stage()


AX = mybir.AxisListType
BIG = 1.0e30


def stage_moe_pre(kb, C, IN, l, x_in, hT_d, gT_d):
    s = kb.s
    kb.begin_stage()
    alloc_norm_scratch(kb, C)
    wr = kb.sb([128, KC, 36], F32)
    rb = kb.sb([128, 36], F32)
    s.dma(SP, wr[:], IN["moe_wr"][l, :, :].rearrange("(kc p) n -> p kc n", p=128), R=[IN["moe_wr"]], W=[wr])
    s.dma(SP, rb[:], IN["moe_rb"][l, :, :], R=[IN["moe_rb"]], W=[rb])
    xb = [kb.sb([128, KC, 512], F32) for _ in range(2)]
    hf = kb.sb([128, KC, 512], F32)
    hb = [kb.sb([128, KC, 512], BF16) for _ in range(2)]
    gts = [kb.sb([32, 512], BF16) for _ in range(2)]
    lgp = [kb.ps([128, 36]) for _ in range(2)]
    gtp = [kb.ps([32, 128]) for _ in range(2)]
    ident = C["ident_f"]

    def small(shape):
        return [kb.sb(shape, F32) for _ in range(2)]

    lgs, gmax, gsel, nmax, ex, gsum, pen = small([128, 36]), small([128, 1]), small([128, 4]), small([128, 1]), small([128, 4]), small([128, 1]), small([128, 4])
    elm, m1, oh1, elm2, m2, oh2 = small([128, 32]), small([128, 1]), small([128, 32]), small([128, 32]), small([128, 1]), small([128, 32])
    dd, ed, w1, w2, gate = small([128, 1]), small([128, 1]), small([128, 1]), small([128, 1]), small([128, 32])
    a_col = (C["a2"], l * 8)
    sh_col = mod_col(C, l, "sh2")
    it = 0
    for g in range(NG):
        xg = xb[g % 2]
        hT = hb[g % 2]
        gt = gts[g % 2]
        tsl = slice(g * 512, (g + 1) * 512)
        s.dma(SP, xg[:], x_in[:, tsl].rearrange("(kc p) t -> p kc t", p=128), R=[x_in], W=[xg])
        norm_group(kb, C, xg, a_col, sh_col, hT, hT_f32=hf)
        s.dma(SP, hT_d[:, tsl].rearrange("(kc p) t -> p kc t", p=128), hT[:], R=[hT], W=[hT_d])
        for j in range(4):
            i = it % 2
            it += 1
            lp = lgp[i]
            for kc in range(KC):
                s.op(PE, lambda e, kc=kc: e.matmul(lp[:], lhsT=hf[:, kc, j * 128:(j + 1) * 128], rhs=wr[:, kc, :],
                                                    start=(kc == 0), stop=(kc == KC - 1)), R=[hf, wr], W=[lp])
            L, GM, GS, NM, EX, GSUM, PEN = lgs[i], gmax[i], gsel[i], nmax[i], ex[i], gsum[i], pen[i]
            ELM, M1, OH1, ELM2, M2, OH2 = elm[i], m1[i], oh1[i], elm2[i], m2[i], oh2[i]
            DD, ED, W1, W2, G = dd[i], ed[i], w1[i], w2[i], gate[i]
            s.op(DVE, lambda e: e.tensor_tensor(out=L[:], in0=lp[:], in1=rb[:], op=ALU.add), R=[lp, rb], W=[L])
            s.op(DVE, lambda e: e.reduce_max(out=GM[:], in_=L[:, 0:4], axis=AX.X), R=[L], W=[GM])
            s.op(DVE, lambda e: e.tensor_scalar(out=GS[:], in0=L[:, 0:4], scalar1=GM[:, 0:1], scalar2=None,
                                                op0=ALU.is_ge), R=[L, GM], W=[GS])
            s.op(DVE, lambda e: e.tensor_scalar(out=NM[:], in0=GM[:], scalar1=-1.0, scalar2=None, op0=ALU.mult),
                 R=[GM], W=[NM])
            s.op(ACT, lambda e: e.activation(out=EX[:], in_=L[:, 0:4], func=AF.Exp, bias=NM[:, 0:1],
                                             accum_out=GSUM[:, 0:1]), R=[L, NM], W=[EX, GSUM])
            s.op(DVE, lambda e: e.reciprocal(out=GSUM[:], in_=GSUM[:]), R=[GSUM], W=[GSUM])
            s.op(DVE, lambda e: e.tensor_scalar(out=PEN[:], in0=GS[:], scalar1=-1.0, scalar2=BIG,
                                                op0=ALU.add, op1=ALU.mult), R=[GS], W=[PEN])
            for gi in range(4):
                s.op(DVE, lambda e, gi=gi: e.tensor_scalar(out=ELM[:, gi * 8:(gi + 1) * 8],
                                                           in0=L[:, 4 + gi * 8:4 + (gi + 1) * 8],
                                                           scalar1=PEN[:, gi:gi + 1], scalar2=None, op0=ALU.add),
                     R=[L, PEN], W=[ELM])
            s.op(DVE, lambda e: e.reduce_max(out=M1[:], in_=ELM[:], axis=AX.X), R=[ELM], W=[M1])
            s.op(DVE, lambda e: e.tensor_scalar(out=OH1[:], in0=ELM[:], scalar1=M1[:, 0:1], scalar2=None,
                                                op0=ALU.is_ge), R=[ELM, M1], W=[OH1])
            s.op(DVE, lambda e: e.scalar_tensor_tensor(out=ELM2[:], in0=OH1[:], scalar=-BIG, in1=ELM[:],
                                                       op0=ALU.mult, op1=ALU.add), R=[OH1, ELM], W=[ELM2])
            s.op(DVE, lambda e: e.reduce_max(out=M2[:], in_=ELM2[:], axis=AX.X), R=[ELM2], W=[M2])
            s.op(DVE, lambda e: e.tensor_scalar(out=OH2[:], in0=ELM2[:], scalar1=M2[:, 0:1], scalar2=None,
                                                op0=ALU.is_ge), R=[ELM2, M2], W=[OH2])
            s.op(DVE, lambda e: e.tensor_tensor(out=DD[:], in0=M2[:], in1=M1[:], op=ALU.subtract), R=[M1, M2], W=[DD])
            s.op(ACT, lambda e: e.activation(out=ED[:], in_=DD[:], func=AF.Exp), R=[DD], W=[ED])
            s.op(DVE, lambda e: e.tensor_scalar(out=W1[:], in0=ED[:], scalar1=1.0, scalar2=None, op0=ALU.add),
                 R=[ED], W=[W1])
            s.op(DVE, lambda e: e.reciprocal(out=W1[:], in_=W1[:]), R=[W1], W=[W1])
            s.op(DVE, lambda e: e.tensor_tensor(out=W2[:], in0=ED[:], in1=W1[:], op=ALU.mult), R=[ED, W1], W=[W2])
            s.op(DVE, lambda e: e.tensor_tensor(out=W1[:], in0=W1[:], in1=GSUM[:], op=ALU.mult), R=[W1, GSUM], W=[W1])
            s.op(DVE, lambda e: e.tensor_tensor(out=W2[:], in0=W2[:], in1=GSUM[:], op=ALU.mult), R=[W2, GSUM], W=[W2])
            s.op(DVE, lambda e: e.tensor_scalar(out=G[:], in0=OH1[:], scalar1=W1[:, 0:1], scalar2=None, op0=ALU.mult),
                 R=[OH1, W1], W=[G])
            s.op(DVE, lambda e: e.scalar_tensor_tensor(out=G[:], in0=OH2[:], scalar=W2[:, 0:1], in1=G[:],
                                                       op0=ALU.mult, op1=ALU.add), R=[OH2, W2, G], W=[G])
            gp = gtp[i]
            s.op(PE, lambda e: e.matmul(gp[:], lhsT=G[:], rhs=ident[:], start=True, stop=True), R=[G, ident], W=[gp])
            s.op(ACT, lambda e, j=j: e.activation(out=gt[:, j * 128:(j + 1) * 128], in_=gp[:], func=AF.Identity),
                 R=[gp], W=[gt])
        s.dma(SP, gT_d[:, tsl], gt[:], R=[gt], W=[gT_d])
    kb.end_stage()


SBK = 2048
NSB = T // SBK


def stage_moe_exp(kb, C, IN, l, x_in, x_out, hT_d, gT_d):
    s = kb.s
    kb.begin_stage()
    sel = kb.sb([128, N_EXP * 128], BF16)
    s.dma(POOL, sel[:], IN["sel"][:, :], R=[IN["sel"]], W=[sel])
    gT = kb.sb([128, SBK], BF16)
    s.op(DVE, lambda e: e.memset(gT[:], 0.0), W=[gT])
    hT = [kb.sb([128, KC, 512], BF16) for _ in range(4)]
    yacc = [kb.sb([128, KC, 512], F32) for _ in range(4)]
    wg = [kb.sb([128, KC, DFF], BF16) for _ in range(2)]
    wu = [kb.sb([128, KC, DFF], BF16) for _ in range(2)]
    wd = [kb.sb([128, 4, D], BF16) for _ in range(2)]
    Ab = [kb.sb([128, 4, 512], BF16) for _ in range(2)]
    Sb = [kb.sb([128, 512], F32) for _ in range(2)]
    Tb = [kb.sb([128, 512], F32) for _ in range(2)]
    xg = kb.sb([128, KC, 512], F32)
    gbps = [kb.ps([128, 512]) for _ in range(2)]
    Gp = [kb.ps([128, 512]) for _ in range(2)]
    Up = [kb.ps([128, 512]) for _ in range(2)]
    Yp = [kb.ps([128, 512]) for _ in range(2)]
    modt = C["mod"]
    g2o = l * 48 + 40
    wi = 0
    ci = 0
    yi = 0
    for sb in range(NSB):
        t0 = sb * SBK
        s.dma(SP, gT[0:32, :], gT_d[:, t0:t0 + SBK], R=[gT_d], W=[gT])
        for g in range(4):
            s.dma(SP, hT[g][:], hT_d[:, t0 + g * 512:t0 + (g + 1) * 512].rearrange("(kc p) t -> p kc t", p=128),
                  R=[hT_d], W=[hT[g]])
        for ex in range(N_EXP):
            b = wi % 2
            wi += 1
            WG, WU, WD = wg[b], wu[b], wd[b]
            s.dma(POOL, WG[:], IN["moe_w_gate"][l, ex, :, :].rearrange("(kc p) f -> p kc f", p=128),
                  R=[IN["moe_w_gate"]], W=[WG])
            s.dma(POOL, WU[:], IN["moe_w_up"][l, ex, :, :].rearrange("(kc p) f -> p kc f", p=128),
                  R=[IN["moe_w_up"]], W=[WU])
            s.dma(POOL, WD[:], IN["moe_w_down"][l, ex, :, :].rearrange("(fc p) n -> p fc n", p=128),
                  R=[IN["moe_w_down"]], W=[WD])

            def gu_phase(g):
                nonlocal ci
                A = Ab[g % 2]
                gbp = gbps[g % 2]
                s.op(PE, lambda e: e.matmul(gbp[:], lhsT=sel[:, ex * 128:(ex + 1) * 128],
                                            rhs=gT[:, g * 512:(g + 1) * 512], start=True, stop=True),
                     R=[sel, gT], W=[gbp])
                for fc in range(4):
                    i = ci % 2
                    ci += 1
                    GP, UP, S_, T_ = Gp[i], Up[i], Sb[i], Tb[i]
                    for kc in range(KC):
                        s.op(PE, lambda e, kc=kc: e.matmul(GP[:], lhsT=WG[:, kc, fc * 128:(fc + 1) * 128],
                                                            rhs=hT[g][:, kc, :], start=(kc == 0), stop=(kc == KC - 1)),
                             R=[WG, hT[g]], W=[GP])
                    for kc in range(KC):
                        s.op(PE, lambda e, kc=kc: e.matmul(UP[:], lhsT=WU[:, kc, fc * 128:(fc + 1) * 128],
                                                            rhs=hT[g][:, kc, :], start=(kc == 0), stop=(kc == KC - 1)),
                             R=[WU, hT[g]], W=[UP])
                    s.op(ACT, lambda e: e.activation(out=S_[:], in_=GP[:], func=AF.Silu), R=[GP], W=[S_])
                    s.op(DVE, lambda e: e.tensor_tensor(out=T_[:], in0=UP[:], in1=S_[:], op=ALU.mult),
                         R=[UP, S_], W=[T_])
                    s.op(DVE, lambda e: e.tensor_tensor(out=A[:, fc, :], in0=gbp[:], in1=T_[:], op=ALU.mult),
                         R=[gbp, T_], W=[A])

            def y_phase(g):
                nonlocal yi
                A = Ab[g % 2]
                for n in range(KC):
                    YP = Yp[yi % 2]
                    yi += 1
                    for fc in range(4):
                        s.op(PE, lambda e, fc=fc: e.matmul(YP[:], lhsT=WD[:, fc, n * 128:(n + 1) * 128],
                                                            rhs=A[:, fc, :], start=(fc == 0), stop=(fc == 3)),
                             R=[WD, A], W=[YP])
                    if ex == 0:
                        s.op(ACT, lambda e: e.activation(out=yacc[g][:, n, :], in_=YP[:], func=AF.Identity),
                             R=[YP], W=[yacc[g]])
                    else:
                        s.op(DVE, lambda e: e.tensor_tensor(out=yacc[g][:, n, :], in0=YP[:], in1=yacc[g][:, n, :],
                                                            op=ALU.add), R=[YP, yacc[g]], W=[yacc[g]])

            gu_phase(0)
            for g in range(1, 4):
                gu_phase(g)
                y_phase(g - 1)
            y_phase(3)
        for g in range(4):
            tsl = slice(t0 + g * 512, t0 + (g + 1) * 512)
            s.dma(SP, xg[:], x_in[:, tsl].rearrange("(kc p) t -> p kc t", p=128), R=[x_in], W=[xg])
            for n in range(KC):
                s.op(DVE, lambda e, n=n: e.scalar_tensor_tensor(
                    out=yacc[g][:, n, :], in0=yacc[g][:, n, :], scalar=modt[:, g2o + n:g2o + n + 1],
                    in1=xg[:, n, :], op0=ALU.mult, op1=ALU.add), R=[yacc[g], modt, xg], W=[yacc[g]])
            s.dma(SP, x_out[:, tsl].rearrange("(kc p) t -> p kc t", p=128), yacc[g][:], R=[yacc[g]], W=[x_out])
    kb.end_stage()


def input_specs(cfg):
    sp = {
        "xT": ([D, T], F32),
        "c_col": ([128, 8], F32),
        "adab_col": ([128, DEPTH * 48], F32),
        "normg_col": ([128, (2 * DEPTH + 1) * 8], F32),
        "ada_w": ([DEPTH, D, 6 * D], F32),
        "ident": ([128, 128], F32),
    }
    if cfg["moe"]:
        sp.update({
            "moe_wr": ([DEPTH, D, 36], F32),
            "moe_rb": ([DEPTH, 128, 36], F32),
            "sel": ([128, N_EXP * 128], F32),
            "moe_w_gate": ([DEPTH, N_EXP, D, DFF], F32),
            "moe_w_up": ([DEPTH, N_EXP, D, DFF], F32),
            "moe_w_down": ([DEPTH, N_EXP, DFF, D], F32),
        })
    if 0 in cfg["mixers"]:
        sp.update({
            "sb_w_in": ([1, D, 3 * D], F32),
            "sb_w_out": ([1, D, D], F32),
            "ntri": ([128, 128], F32),
            "sbmask": ([128, 4, 512], F32),
        })
    if 3 in cfg["mixers"]:
        sp.update({
            "dil_w_in": ([1, D, 4608], F32),
            "dil_w_out": ([1, 512, D], F32),
            "dilmask": ([128, DIL_NMASK, 512], F32),
            "shift": ([128, 64], F32),
        })
    if 1 in cfg["mixers"]:
        sp.update({
            "gla_w_in": ([1, D, 3088], F32),
            "gla_w_out": ([1, D, D], F32),
            "gla_wgu": ([17, 512], F32),
            "gla_tri": ([128, 128], F32),
            "gla_up": ([128, 128], F32),
            "gla_cmask": ([128, 128], F32),
            "gla_ng_col": ([128, 2], F32),
        })
    if 2 in cfg["mixers"]:
        sp.update({
            "conv_w_in": ([1, D, 3 * D], F32),
            "conv_w_col": ([128, 24], F32),
            "conv_b_col": ([128, 8], F32),
            "conv_w_out": ([1, D, D], F32),
        })
    return sp


def build(cfg):
    nc = bass.Bass("TRN2", target_bir_lowering=False)
    IN = {}
    for name, (shape, dt) in input_specs(cfg).items():
        IN[name] = Tl(nc.dram_tensor(name, list(shape), dt, kind="ExternalInput").ap(), multi=True)
    outT = Tl(nc.dram_tensor("outT", [D, T], F32, kind="ExternalOutput").ap(), multi=True)
    with ExitStack() as stack:
        kb = KB(nc, stack)
        C = {}
        xs = [kb.dram("xs0", [D, T], F32), kb.dram("xs1", [D, T], F32)]
        zT = kb.dram("zT", [D, T], BF16)
        hT_d = kb.dram("hT_d", [D, T], BF16)
        qT_d = kb.dram("qT_d", [D, T], BF16)
        kT_d = kb.dram("kT_d", [D, T], BF16)
        v_d = kb.dram("v_d", [T, D], BF16)
        dq_d = kb.dram("dq_d", [1536, T], BF16)
        gq_d = kb.dram("gq_d", [512, T], BF16)
        gk_d = kb.dram("gk_d", [512, T], BF16)
        gks_d = kb.dram("gks_d", [T, 512], BF16)
        gv_d = kb.dram("gv_d", [T, D], BF16)
        gsr_d = kb.dram("gsr_d", [D, T], BF16)
        gdec_d = kb.dram("gdec_d", [128, 512], F32)
        dk_d = kb.dram("dk_d", [1536, T], BF16)
        dv_d = kb.dram("dv_d", [T, 1536], BF16)
        gT_d = kb.dram("gT_d", [N_EXP, T], BF16)
        stage_consts(kb, C, IN)
        stage_mod(kb, C, IN)
        cur = IN["xT"]
        nxt = 0
        for l in range(DEPTH):
            if l in cfg["mixers"]:
                if l == 0:
                    nh = cfg.get("sb_heads", 16)
                    stage_inproj(kb, C, l, cur, IN["sb_w_in"][0, :, :], 3 * D,
                                 [(0, 8, qT_d, 0, 0.125), (D, 8, kT_d, 0, 1.0)], [(2 * D, D, v_d, 0)])
                    stage_sb_attn(kb, C, IN, qT_d, kT_d, v_d, zT, n_heads=nh)
                    stage_outproj(kb, C, l, zT, KC, IN["sb_w_out"][0, :, :], cur, xs[nxt])
                if l == 3:
                    fm, tm = [], []
                    for dg in range(3):
                        fm.append((dg * 1536, 4, dq_d, dg * 512, 0.125))
                        fm.append((dg * 1536 + 512, 4, dk_d, dg * 512, 1.0))
                        tm.append((dg * 1536 + 1024, 512, dv_d, dg * 512))
                    stage_inproj(kb, C, l, cur, IN["dil_w_in"][0, :, :], 4608, fm, tm)
                    stage_dil_attn(kb, C, IN, dq_d, dk_d, dv_d, zT, n_heads=cfg.get("dil_heads", 8))
                    stage_outproj(kb, C, l, zT, 4, IN["dil_w_out"][0, :, :], cur, xs[nxt])
                if l == 1:
                    stage_gla_proj(kb, C, IN, l, cur, gq_d, gk_d, gks_d, gv_d, gsr_d, gdec_d)
                    stage_gla_scan(kb, C, IN, gq_d, gk_d, gks_d, gv_d, gsr_d, gdec_d, zT)
                    stage_outproj(kb, C, l, zT, KC, IN["gla_w_out"][0, :, :], cur, xs[nxt])
                if l == 2:
                    stage_conv(kb, C, IN, l, cur, zT)
                    stage_outproj(kb, C, l, zT, KC, IN["conv_w_out"][0, :, :], cur, xs[nxt])
                cur = xs[nxt]
                nxt ^= 1
            if l in cfg["moe"]:
                stage_moe_pre(kb, C, IN, l, cur, hT_d, gT_d)
                stage_moe_exp(kb, C, IN, l, cur, xs[nxt], hT_d, gT_d)
                cur = xs[nxt]
                nxt ^= 1
        stage_final(kb, C, cur, outT)
        kb.s.wait_all(SP)
        print("instructions:", kb.s.n_ins, "waits:", kb.s.n_wait, "sems:", len(kb.s.sems))
    return nc


def colvec(v):
    return np.ascontiguousarray(v.reshape(KC, 128).T)


def prep_core_inputs(inputs, b, cfg):
    f = np.float32
    m = {}
    m["xT"] = np.ascontiguousarray(inputs["x"][b].T).astype(f, copy=False)
    m["c_col"] = colvec(inputs["c"][b]).astype(f)
    m["adab_col"] = np.ascontiguousarray(
        np.concatenate([inputs["ada_b"][l].reshape(48, 128).T for l in range(DEPTH)], axis=1)).astype(f)
    ng = [colvec(inputs["norm_g"][l, s_]) for l in range(DEPTH) for s_ in range(2)]
    ng.append(colvec(inputs["final_g"]))
    m["normg_col"] = np.ascontiguousarray(np.concatenate(ng, axis=1)).astype(f)
    m["ada_w"] = inputs["ada_w"]
    m["ident"] = np.eye(128, dtype=f)
    if cfg["moe"]:
        m["moe_wr"] = np.ascontiguousarray(np.concatenate([inputs["moe_w_grp"], inputs["moe_w_exp"]], axis=2)).astype(f)
        rb = np.concatenate([inputs["moe_b_grp"], inputs["moe_b_exp"]], axis=1)
        m["moe_rb"] = np.ascontiguousarray(np.broadcast_to(rb[:, None, :], (DEPTH, 128, 36))).astype(f)
        sel = np.zeros((128, N_EXP * 128), dtype=f)
        for e_ in range(N_EXP):
            sel[e_, e_ * 128:(e_ + 1) * 128] = 1.0
        m["sel"] = sel
        m["moe_w_gate"] = inputs["moe_w_gate"]
        m["moe_w_up"] = inputs["moe_w_up"]
        m["moe_w_down"] = inputs["moe_w_down"]
    if 0 in cfg["mixers"]:
        m["sb_w_in"] = inputs["sb_w_in"]
        m["sb_w_out"] = inputs["sb_w_out"]
        jj = np.arange(128)
        m["ntri"] = -(jj[:, None] >= jj[None, :]).astype(f)
        tt = np.arange(512)
        m["sbmask"] = np.ascontiguousarray(np.stack(
            [(tt[None, :] > (128 * i + jj[:, None])).astype(f) for i in range(4)], axis=1))
    if 3 in cfg["mixers"]:
        m["dil_w_in"] = inputs["dil_w_in"]
        m["dil_w_out"] = inputs["dil_w_out"]
        sl = np.arange(128)[:, None]
        tl = np.arange(512)[None, :]
        mk = []
        for (W_, r_) in DIL_PAT:
            for rel in range(W_ // 128 + 4):
                dlt = tl - sl + W_ - 128 * rel
                mk.append(((dlt >= 0) & (dlt <= W_) & (dlt % r_ == 0)).astype(f))
        m["dilmask"] = np.ascontiguousarray(np.stack(mk, axis=1))
        sh = np.zeros((128, 64), dtype=f)
        sh[64 + np.arange(64), np.arange(64)] = 1.0
        m["shift"] = sh
    if 1 in cfg["mixers"]:
        m["gla_w_in"] = inputs["gla_w_in"]
        m["gla_w_out"] = inputs["gla_w_out"]
        m["gla_wgu"] = np.ascontiguousarray(
            np.concatenate([inputs["gla_w_gate_up"][0], inputs["gla_b_gate"][0][None, :]], axis=0)).astype(f)
        ii = np.arange(128)
        same = (ii[:, None] // 64) == (ii[None, :] // 64)
        m["gla_tri"] = (-(1.0 / 16.0) * (same & (ii[:, None] <= ii[None, :]))).astype(f)
        m["gla_up"] = (-(1.0 / 16.0) * (same & (ii[:, None] > ii[None, :]))).astype(f)
        m["gla_cmask"] = (same & (ii[None, :] >= ii[:, None])).astype(f)
        m["gla_ng_col"] = np.ascontiguousarray(inputs["gla_norm_g"][0].reshape(2, 128).T).astype(f)
    if 2 in cfg["mixers"]:
        m["conv_w_in"] = inputs["conv_w_in"]
        m["conv_w_col"] = np.ascontiguousarray(
            np.concatenate([colvec(inputs["conv_w"][0, j]) for j in range(3)], axis=1)).astype(f)
        m["conv_b_col"] = colvec(inputs["conv_b"][0]).astype(f)
        m["conv_w_out"] = inputs["conv_w_out"]
    return m


FULL_CFG = {"mixers": (0, 1, 2, 3), "moe": (0, 1, 2, 3)}


def run(inputs, cfg, n_cores=8, trace=False):
    inputs = {k: np.asarray(v) for k, v in inputs.items()}
    nc = build(cfg)
    in_maps = [prep_core_inputs(inputs, b, cfg) for b in range(n_cores)]
    res = run_bass_kernel_spmd(nc, in_maps, core_ids=list(range(n_cores)), trace=trace)
    out = np.stack([np.ascontiguousarray(r["outT"].T) for r in res.results], axis=0)
    return out.astype(np.float32), res


def kernel(**inputs):
    out, _ = run(inputs, FULL_CFG)
    return out
```

```python
import numpy as np
from contextlib import ExitStack
import concourse.bass as bass
import concourse.mybir as mybir
from concourse.bass_utils import run_bass_kernel_spmd

F32 = mybir.dt.float32
BF16 = mybir.dt.bfloat16
AF = mybir.ActivationFunctionType
ALU = mybir.AluOpType

D = 1024
T = 8192
KC = 8
DEPTH = 4
NG = T // 512
EPS = 1e-6
N_EXP = 32
DFF = 512

SP, ACT, DVE, POOL, PE = "sp", "act", "dve", "pool", "pe"
ALL_ENG = (SP, ACT, DVE, POOL, PE)
COMPUTE = (ACT, DVE, POOL, PE)


class Reg:
    __slots__ = ("w", "r", "multi")

    def __init__(self, multi=False):
        self.w = {}
        self.r = {}
        self.multi = multi


class Tl:
    def __init__(self, h, multi=False):
        self.h = h
        self.reg = Reg(multi)

    def __getitem__(self, idx):
        return self.h[idx]


class Sched:
    GEN = 30000
    NDMA = 8
    NATTACH = 1

    def __init__(self, nc, stack):
        self.nc = nc
        self.stack = stack
        self.eng = {SP: nc.sync, ACT: nc.scalar, DVE: nc.vector, POOL: nc.gpsimd, PE: nc.tensor}
        self.sems = []
        self.seq = {e: 0 for e in COMPUTE}
        self.esem = {e: [] for e in COMPUTE}
        self.own = {e: set() for e in ALL_ENG}
        self.waited = {e: {} for e in ALL_ENG}
        self.dsem = {}
        self.dnext = {}
        self.dval = {}
        self.n_ins = 0
        self.n_wait = 0
        for q in (SP, ACT, POOL):
            self.dsem[q] = [self._newsem("d%s%d" % (q, i)) for i in range(self.NDMA)]
            self.dnext[q] = 0
            for s in self.dsem[q]:
                self.dval[s] = 0

    def _newsem(self, name):
        h = self.stack.enter_context(self.nc.semaphore(name))
        self.sems.append(h)
        return len(self.sems) - 1

    def _deps(self, eng, reads, writes):
        deps = {}

        def add(s, v, raw):
            o = deps.get(s)
            if o is None:
                deps[s] = [v, raw]
            else:
                if v > o[0]:
                    o[0] = v
                o[1] = o[1] or raw

        for r in reads:
            for s, v in r.reg.w.items():
                add(s, v, True)
        for w in writes:
            for s, v in w.reg.r.items():
                add(s, v, False)
            if not w.reg.multi:
                for s, v in w.reg.w.items():
                    add(s, v, False)
        waits = []
        own = self.own[eng]
        wd = self.waited[eng]
        for s, (v, raw) in deps.items():
            if s in own:
                if eng == PE or not raw:
                    continue
            if wd.get(s, 0) >= v:
                continue
            wd[s] = v
            waits.append((s, v))
        return waits

    def _update(self, reads, writes, s, val):
        for r in reads:
            if r in writes:
                continue
            rr = r.reg.r
            if rr.get(s, 0) < val:
                rr[s] = val
        for w in writes:
            g = w.reg
            if g.r:
                g.w = {s: val}
                g.r = {}
            else:
                if g.w.get(s, 0) < val:
                    g.w[s] = val

    def _emit(self, eng, waits, fn, s, inc, attach_ok=False):
        e = self.eng[eng]
        attach = []
        if fn is not None and waits and attach_ok:
            attach = waits[-self.NATTACH:]
            waits = waits[:-self.NATTACH]
        for (ws, wv) in waits:
            e.wait_ge(self.sems[ws], wv)
            self.n_wait += 1
        if fn is not None:
            ins = fn(e)
            for (ws, wv) in attach:
                ins._wait_ge(self.sems[ws], wv)
            ins.then_inc(self.sems[s], inc)
            self.n_ins += 1

    def op(self, eng, fn, R=(), W=()):
        waits = self._deps(eng, R, W)
        self.seq[eng] += 1
        q = self.seq[eng]
        g = (q - 1) // self.GEN
        while len(self.esem[eng]) <= g:
            s_new = self._newsem("e%s%d" % (eng, len(self.esem[eng])))
            self.esem[eng].append(s_new)
            self.own[eng].add(s_new)
        s = self.esem[eng][g]
        val = q - g * self.GEN
        self._emit(eng, waits, fn, s, 1, attach_ok=True)
        self._update(R, W, s, val)

    def dma(self, eng, out, in_, R=(), W=()):
        waits = self._deps(eng, R, W)
        i = self.dnext[eng]
        self.dnext[eng] = (i + 1) % self.NDMA
        s = self.dsem[eng][i]
        prev = self.dval[s]
        wd = self.waited[eng]
        if prev > 0 and wd.get(s, 0) < prev:
            wd[s] = prev
            waits.append((s, prev))
        val = prev + 16
        self.dval[s] = val
        self._emit(eng, waits, lambda e: e.dma_start(out=out, in_=in_), s, 16)
        self._update(R, W, s, val)

    def _latest(self):
        lat = {}
        for e in COMPUTE:
            q = self.seq[e]
            if q > 0:
                g = (q - 1) // self.GEN
                lat[self.esem[e][g]] = q - g * self.GEN
        for s, v in self.dval.items():
            if v > 0:
                lat[s] = v
        return lat

    def barrier(self):
        lat = self._latest()
        for eng in ALL_ENG:
            wd = self.waited[eng]
            e = self.eng[eng]
            for s, v in lat.items():
                if eng in COMPUTE and s in self.own[eng]:
                    pass
                if wd.get(s, 0) >= v:
                    continue
                wd[s] = v
                e.wait_ge(self.sems[s], v)
                self.n_wait += 1

    def wait_all(self, eng):
        lat = self._latest()
        wd = self.waited[eng]
        e = self.eng[eng]
        for s, v in lat.items():
            if wd.get(s, 0) >= v:
                continue
            wd[s] = v
            e.wait_ge(self.sems[s], v)


class KB:
    def __init__(self, nc, stack):
        self.nc = nc
        self.gstack = stack
        self.s = Sched(nc, stack)
        self.stage_stack = None
        self._n = 0

    def name(self, p):
        self._n += 1
        return "%s_%d" % (p, self._n)

    def begin_stage(self):
        self.s.barrier()
        self.stage_stack = ExitStack()
        self.stage_stack.__enter__()

    def end_stage(self):
        self.s.barrier()
        self.stage_stack.__exit__(None, None, None)
        self.stage_stack = None

    def sb(self, shape, dtype, glob=False, multi=False):
        st = self.gstack if glob else self.stage_stack
        h = st.enter_context(self.nc.sbuf_tensor(self.name("sb"), list(shape), dtype))
        return Tl(h, multi)

    def ps(self, shape, dtype=F32, glob=False):
        st = self.gstack if glob else self.stage_stack
        h = st.enter_context(self.nc.psum_tensor(self.name("ps"), list(shape), dtype))
        return Tl(h)

    def dram(self, name, shape, dtype):
        h = self.nc.dram_tensor(name, list(shape), dtype)
        return Tl(h.ap(), multi=True)


def colview(ap2d):
    return ap2d.rearrange("(kc p) n -> p kc n", p=128)


def load_w_bf16(kb, dst, src_ap, R):
    kb.s.dma(POOL, dst_ap(dst), src_ap, R=R, W=[dst])


def dst_ap(t):
    return t.h[:]


def norm_group(kb, C, xg, a_col, sh_col, hT_bf, hT_f32=None):
    s = kb.s
    sq, ssp, rstd, tmp = C["n_sq"], C["n_ssp"], C["n_rstd"], C["n_tmp"]
    ones = C["ones_bf"]
    s.op(ACT, lambda e: e.activation(out=sq[:], in_=xg[:], func=AF.Square, scale=1.0 / 32.0),
         R=[xg], W=[sq])
    for kc in range(KC):
        s.op(PE, lambda e, kc=kc: e.matmul(ssp[:], lhsT=ones[:], rhs=sq[:, kc, :],
                                            start=(kc == 0), stop=(kc == KC - 1)),
             R=[ones, sq], W=[ssp])
    s.op(ACT, lambda e: e.activation(out=rstd[:], in_=ssp[:], func=AF.Sqrt, bias=C["eps_col"][:, 0:1]),
         R=[ssp, C["eps_col"]], W=[rstd])
    s.op(DVE, lambda e: e.reciprocal(out=rstd[:], in_=rstd[:]), R=[rstd], W=[rstd])
    at, ao = a_col
    st_, so = sh_col
    for kc in range(KC):
        s.op(DVE, lambda e, kc=kc: e.tensor_tensor(out=tmp[:, kc, :], in0=xg[:, kc, :], in1=rstd[:],
                                                    op=ALU.mult),
             R=[xg, rstd], W=[tmp])
    for kc in range(KC):
        if hT_f32 is not None:
            s.op(ACT, lambda e, kc=kc: e.activation(out=hT_f32[:, kc, :], in_=tmp[:, kc, :],
                                                     func=AF.Identity,
                                                     scale=at[:, ao + kc:ao + kc + 1],
                                                     bias=st_[:, so + kc:so + kc + 1]),
                 R=[tmp, at, st_], W=[hT_f32])
        else:
            s.op(ACT, lambda e, kc=kc: e.activation(out=hT_bf[:, kc, :], in_=tmp[:, kc, :],
                                                     func=AF.Identity,
                                                     scale=at[:, ao + kc:ao + kc + 1],
                                                     bias=st_[:, so + kc:so + kc + 1]),
                 R=[tmp, at, st_], W=[hT_bf])
    if hT_f32 is not None:
        s.op(POOL, lambda e: e.tensor_copy(out=hT_bf[:], in_=hT_f32[:]), R=[hT_f32], W=[hT_bf])


def alloc_norm_scratch(kb, C):
    C["n_sq"] = kb.sb([128, KC, 512], BF16)
    C["n_ssp"] = kb.ps([128, 512])
    C["n_rstd"] = kb.sb([128, 512], F32)
    C["n_tmp"] = kb.sb([128, KC, 512], F32)


def stage_consts(kb, C, IN):
    s = kb.s
    nc = kb.nc
    C["ones_bf"] = kb.sb([128, 128], BF16, glob=True)
    C["eps_col"] = kb.sb([128, 1], F32, glob=True)
    C["ident_f"] = kb.sb([128, 128], F32, glob=True)
    s.op(DVE, lambda e: e.memset(C["ones_bf"][:], 1.0), W=[C["ones_bf"]])
    s.op(DVE, lambda e: e.memset(C["eps_col"][:], EPS), W=[C["eps_col"]])
    s.dma(SP, C["ident_f"][:], IN["ident"][:, :], R=[IN["ident"]], W=[C["ident_f"]])
    C["mod"] = kb.sb([128, DEPTH * 48], F32, glob=True)
    C["a1"] = kb.sb([128, DEPTH * 8], F32, glob=True)
    C["a2"] = kb.sb([128, DEPTH * 8], F32, glob=True)
    C["normg"] = kb.sb([128, (DEPTH * 2 + 1) * 8], F32, glob=True)
    s.dma(SP, C["normg"][:], IN["normg_col"][:, :], R=[IN["normg_col"]], W=[C["normg"]])


def stage_mod(kb, C, IN):
    s = kb.s
    kb.begin_stage()
    ccol = kb.sb([128, 8], F32)
    cond = kb.sb([128, 8], F32)
    adab = kb.sb([128, DEPTH * 48], F32)
    s.dma(SP, ccol[:], IN["c_col"][:, :], R=[IN["c_col"]], W=[ccol])
    s.dma(SP, adab[:], IN["adab_col"][:, :], R=[IN["adab_col"]], W=[adab])
    s.op(ACT, lambda e: e.activation(out=cond[:], in_=ccol[:], func=AF.Silu), R=[ccol], W=[cond])
    wbuf = [kb.sb([128, KC, 1024], F32) for _ in range(2)]
    pss = [kb.ps([128, 8]) for _ in range(2)]
    it = 0
    for l in range(DEPTH):
        for j6 in range(6):
            wb = wbuf[it % 2]
            pst = pss[it % 2]
            it += 1
            src = IN["ada_w"][l, :, j6 * 1024:(j6 + 1) * 1024].rearrange("(kc p) n -> p kc n", p=128)
            s.dma(SP, wb[:], src, R=[IN["ada_w"]], W=[wb])
            for jn in range(8):
                for kc in range(KC):
                    s.op(PE, lambda e, wb=wb, pst=pst, jn=jn, kc=kc: e.matmul(
                        pst[:, jn:jn + 1], lhsT=wb[:, kc, jn * 128:(jn + 1) * 128],
                        rhs=cond[:, kc:kc + 1], start=(kc == 0), stop=(kc == KC - 1)),
                        R=[wb, cond], W=[pst])
            s.op(DVE, lambda e, pst=pst, l=l, j6=j6: e.tensor_tensor(
                out=C["mod"][:, l * 48 + j6 * 8:l * 48 + (j6 + 1) * 8], in0=pst[:],
                in1=adab[:, l * 48 + j6 * 8:l * 48 + (j6 + 1) * 8], op=ALU.add),
                R=[pst, adab], W=[C["mod"]])
    for l in range(DEPTH):
        for (dst, sub, off) in ((C["a1"], 0, 8), (C["a2"], 1, 32)):
            s.op(DVE, lambda e, dst=dst, sub=sub, off=off, l=l: e.scalar_tensor_tensor(
                out=dst[:, l * 8:(l + 1) * 8], in0=C["mod"][:, l * 48 + off:l * 48 + off + 8], scalar=1.0,
                in1=C["normg"][:, (l * 2 + sub) * 8:(l * 2 + sub + 1) * 8], op0=ALU.add, op1=ALU.mult),
                R=[C["mod"], C["normg"]], W=[dst])
    kb.end_stage()


def mod_col(C, l, which):
    off = {"sh1": 0, "sc1": 8, "g1": 16, "sh2": 24, "sc2": 32, "g2": 40}[which]
    return (C["mod"], l * 48 + off)


def flat_mod(C):
    return C["mod"]


def stage_outproj(kb, C, l, zT, KZ, w_out_ap, x_in, x_out):
    s = kb.s
    kb.begin_stage()
    w = kb.sb([128, KZ, D], BF16)
    s.dma(POOL, w[:], w_out_ap.rearrange("(kc p) n -> p kc n", p=128), R=[], W=[w])
    zb = [kb.sb([128, KZ, 512], BF16) for _ in range(2)]
    xb = [kb.sb([128, KC, 512], F32) for _ in range(2)]
    ob = [kb.sb([128, KC, 512], F32) for _ in range(2)]
    pss = [kb.ps([128, 512]) for _ in range(4)]
    modt = C["mod"]
    g1o = l * 48 + 16
    pi = 0
    for g in range(NG):
        z = zb[g % 2]
        xg = xb[g % 2]
        og = ob[g % 2]
        tsl = slice(g * 512, (g + 1) * 512)
        s.dma(SP, z[:], zT[0:KZ * 128, tsl].rearrange("(kc p) t -> p kc t", p=128), R=[zT], W=[z])
        s.dma(SP, xg[:], x_in[:, tsl].rearrange("(kc p) t -> p kc t", p=128), R=[x_in], W=[xg])
        for n in range(KC):
            pst = pss[pi % 4]
            pi += 1
            for kc in range(KZ):
                s.op(PE, lambda e, pst=pst, kc=kc, n=n, z=z: e.matmul(
                    pst[:], lhsT=w[:, kc, n * 128:(n + 1) * 128], rhs=z[:, kc, :],
                    start=(kc == 0), stop=(kc == KZ - 1)), R=[w, z], W=[pst])
            s.op(DVE, lambda e, pst=pst, n=n, xg=xg, og=og: e.scalar_tensor_tensor(
                out=og[:, n, :], in0=pst[:], scalar=modt[:, g1o + n:g1o + n + 1], in1=xg[:, n, :],
                op0=ALU.mult, op1=ALU.add), R=[pst, modt, xg], W=[og])
        s.dma(SP, x_out[:, tsl].rearrange("(kc p) t -> p kc t", p=128), og[:], R=[og], W=[x_out])
    kb.end_stage()


def stage_conv(kb, C, IN, l, x_in, zT):
    s = kb.s
    kb.begin_stage()
    alloc_norm_scratch(kb, C)
    w = kb.sb([128, KC, 3 * D], BF16)
    for j in range(3):
        s.dma(POOL, w[:, :, j * D:(j + 1) * D],
              IN["conv_w_in"][0, :, j * D:(j + 1) * D].rearrange("(kc p) n -> p kc n", p=128),
              R=[IN["conv_w_in"]], W=[w])
    cw = kb.sb([128, 24], F32)
    cb = kb.sb([128, 8], F32)
    s.dma(SP, cw[:], IN["conv_w_col"][:, :], R=[IN["conv_w_col"]], W=[cw])
    s.dma(SP, cb[:], IN["conv_b_col"][:, :], R=[IN["conv_b_col"]], W=[cb])
    xb = [kb.sb([128, KC, 512], F32) for _ in range(1)]
    hb = [kb.sb([128, KC, 512], BF16) for _ in range(1)]
    up = [kb.sb([128, KC, 514], F32) for _ in range(2)]
    yt = C["n_tmp"]
    zb = [kb.sb([128, KC, 512], BF16) for _ in range(2)]
    gcs = kb.sb([128, 512], F32)
    pss = [kb.ps([128, 512]) for _ in range(4)]
    a_col = (C["a1"], l * 8)
    sh_col = mod_col(C, l, "sh1")
    s.op(DVE, lambda e: e.memset(up[0][:, :, 0:2], 0.0), W=[up[0]])
    pi = 0
    for g in range(NG):
        xg = xb[0]
        hT = hb[0]
        u = up[g % 2]
        un = up[(g + 1) % 2]
        z = zb[g % 2]
        tsl = slice(g * 512, (g + 1) * 512)
        s.dma(SP, xg[:], x_in[:, tsl].rearrange("(kc p) t -> p kc t", p=128), R=[x_in], W=[xg])
        norm_group(kb, C, xg, a_col, sh_col, hT)

        def proj(n_off, n, pst):
            for kc in range(KC):
                s.op(PE, lambda e, kc=kc: e.matmul(
                    pst[:], lhsT=w[:, kc, n_off + n * 128:n_off + (n + 1) * 128], rhs=hT[:, kc, :],
                    start=(kc == 0), stop=(kc == KC - 1)), R=[w, hT], W=[pst])

        for n in range(KC):
            p1 = pss[pi % 4]; pi += 1
            proj(D, n, p1)
            s.op(ACT, lambda e, p1=p1: e.activation(out=gcs[:], in_=p1[:], func=AF.Identity),
                 R=[p1], W=[gcs])
            p2 = pss[pi % 4]; pi += 1
            proj(2 * D, n, p2)
            s.op(DVE, lambda e, p2=p2, n=n, u=u: e.tensor_tensor(
                out=u[:, n, 2:514], in0=p2[:], in1=gcs[:], op=ALU.mult), R=[p2, gcs], W=[u])
        if g + 1 < NG:
            s.op(POOL, lambda e, u=u, un=un: e.tensor_copy(out=un[:, :, 0:2], in_=u[:, :, 512:514]),
                 R=[u], W=[un])
        for n in range(KC):
            s.op(ACT, lambda e, n=n, u=u: e.activation(
                out=yt[:, n, :], in_=u[:, n, 2:514], func=AF.Identity,
                scale=cw[:, 16 + n:16 + n + 1], bias=cb[:, n:n + 1]), R=[u, cw, cb], W=[yt])
            s.op(DVE, lambda e, n=n, u=u: e.scalar_tensor_tensor(
                out=yt[:, n, :], in0=u[:, n, 1:513], scalar=cw[:, 8 + n:8 + n + 1], in1=yt[:, n, :],
                op0=ALU.mult, op1=ALU.add), R=[u, cw, yt], W=[yt])
            s.op(DVE, lambda e, n=n, u=u: e.scalar_tensor_tensor(
                out=yt[:, n, :], in0=u[:, n, 0:512], scalar=cw[:, n:n + 1], in1=yt[:, n, :],
                op0=ALU.mult, op1=ALU.add), R=[u, cw, yt], W=[yt])
            p3 = pss[pi % 4]; pi += 1
            proj(0, n, p3)
            s.op(DVE, lambda e, p3=p3, n=n, z=z: e.tensor_tensor(
                out=z[:, n, :], in0=p3[:], in1=yt[:, n, :], op=ALU.mult), R=[p3, yt], W=[z])
        s.dma(SP, zT[:, tsl].rearrange("(kc p) t -> p kc t", p=128), z[:], R=[z], W=[zT])
    kb.end_stage()


def stage_final(kb, C, x_in, out):
    s = kb.s
    kb.begin_stage()
    alloc_norm_scratch(kb, C)
    xb = [kb.sb([128, KC, 512], F32) for _ in range(2)]
    ob = [kb.sb([128, KC, 512], F32) for _ in range(2)]
    sq, ssp, rstd = C["n_sq"], C["n_ssp"], C["n_rstd"]
    ones = C["ones_bf"]
    fg = C["normg"]
    for g in range(NG):
        xg = xb[g % 2]
        og = ob[g % 2]
        tsl = slice(g * 512, (g + 1) * 512)
        s.dma(SP, xg[:], x_in[:, tsl].rearrange("(kc p) t -> p kc t", p=128), R=[x_in], W=[xg])
        s.op(ACT, lambda e, xg=xg: e.activation(out=sq[:], in_=xg[:], func=AF.Square, scale=1.0 / 32.0),
             R=[xg], W=[sq])
        for kc in range(KC):
            s.op(PE, lambda e, kc=kc: e.matmul(ssp[:], lhsT=ones[:], rhs=sq[:, kc, :],
                                                start=(kc == 0), stop=(kc == KC - 1)),
                 R=[ones, sq], W=[ssp])
        s.op(ACT, lambda e: e.activation(out=rstd[:], in_=ssp[:], func=AF.Sqrt, bias=C["eps_col"][:, 0:1]),
             R=[ssp, C["eps_col"]], W=[rstd])
        s.op(DVE, lambda e: e.reciprocal(out=rstd[:], in_=rstd[:]), R=[rstd], W=[rstd])
        for kc in range(KC):
            s.op(DVE, lambda e, kc=kc, xg=xg, og=og: e.scalar_tensor_tensor(
                out=og[:, kc, :], in0=xg[:, kc, :], scalar=fg[:, 2 * DEPTH * 8 + kc:2 * DEPTH * 8 + kc + 1], in1=rstd[:],
                op0=ALU.mult, op1=ALU.mult), R=[xg, fg, rstd], W=[og])
        s.dma(SP, out[:, tsl].rearrange("(kc p) t -> p kc t", p=128), og[:], R=[og], W=[out])
    kb.end_stage()


def stage_inproj(kb, C, l, x_in, w_ap, ncols, fm_specs, tm_specs):
    s = kb.s
    kb.begin_stage()
    alloc_norm_scratch(kb, C)
    w = kb.sb([128, KC, ncols], BF16)
    for c0 in range(0, ncols, 1024):
        c1 = min(ncols, c0 + 1024)
        s.dma(POOL, w[:, :, c0:c1], w_ap[:, c0:c1].rearrange("(kc p) n -> p kc n", p=128), R=[], W=[w])
    xg = kb.sb([128, KC, 512], F32)
    hT = kb.sb([128, KC, 512], BF16)
    fo = [kb.sb([128, 512], BF16) for _ in range(3)]
    to = [kb.sb([128, 512], BF16) for _ in range(3)]
    pss = [kb.ps([128, 512]) for _ in range(4)]
    a_col = (C["a1"], l * 8)
    sh_col = mod_col(C, l, "sh1")
    pi = 0
    fi = 0
    ti = 0
    for g in range(NG):
        tsl = slice(g * 512, (g + 1) * 512)
        s.dma(SP, xg[:], x_in[:, tsl].rearrange("(kc p) t -> p kc t", p=128), R=[x_in], W=[xg])
        norm_group(kb, C, xg, a_col, sh_col, hT)
        for (coff, nch, dst, roff, scale) in fm_specs:
            for n in range(nch):
                pst = pss[pi % 4]; pi += 1
                o = fo[fi % 3]; fi += 1
                for kc in range(KC):
                    s.op(PE, lambda e, kc=kc: e.matmul(pst[:], lhsT=w[:, kc, coff + n * 128:coff + (n + 1) * 128],
                                                        rhs=hT[:, kc, :], start=(kc == 0), stop=(kc == KC - 1)),
                         R=[w, hT], W=[pst])
                if (pi % 2) == 0:
                    s.op(ACT, lambda e: e.activation(out=o[:], in_=pst[:], func=AF.Identity, scale=float(scale)),
                         R=[pst], W=[o])
                else:
                    s.op(DVE, lambda e: e.tensor_scalar(out=o[:], in0=pst[:], scalar1=float(scale), scalar2=None,
                                                        op0=ALU.mult), R=[pst], W=[o])
                s.dma(SP, dst[roff + n * 128:roff + (n + 1) * 128, tsl], o[:], R=[o], W=[dst])
        for (coff, ncl, dst, doff) in tm_specs:
            for j in range(4):
                for c in range(ncl // 512):
                    pst = pss[pi % 4]; pi += 1
                    o = to[ti % 3]; ti += 1
                    for kc in range(KC):
                        s.op(PE, lambda e, kc=kc: e.matmul(pst[:], lhsT=hT[:, kc, j * 128:(j + 1) * 128],
                                                            rhs=w[:, kc, coff + c * 512:coff + (c + 1) * 512],
                                                            start=(kc == 0), stop=(kc == KC - 1)),
                             R=[w, hT], W=[pst])
                    if (pi % 2) == 0:
                        s.op(ACT, lambda e: e.activation(out=o[:], in_=pst[:], func=AF.Identity), R=[pst], W=[o])
                    else:
                        s.op(DVE, lambda e: e.tensor_copy(out=o[:], in_=pst[:]), R=[pst], W=[o])
                    s.dma(SP, dst[g * 512 + j * 128:g * 512 + (j + 1) * 128, doff + c * 512:doff + (c + 1) * 512],
                          o[:], R=[o], W=[dst])
    kb.end_stage()


def stage_sb_attn(kb, C, IN, qT_d, kT_d, v_d, oT_d, n_heads=16):
    s = kb.s
    kb.begin_stage()
    ones = C["ones_bf"]
    ntri = kb.sb([128, 128], BF16)
    s.dma(POOL, ntri[:], IN["ntri"][:, :], R=[IN["ntri"]], W=[ntri])
    masks = kb.sb([128, 4, 512], BF16)
    s.dma(POOL, masks[:], IN["sbmask"][:, :, :], R=[IN["sbmask"]], W=[masks])
    qh = [kb.sb([128, T], BF16) for _ in range(2)]
    kh = [kb.sb([128, T], BF16) for _ in range(2)]
    vh = [kb.sb([128, 64, 128], BF16) for _ in range(2)]
    for b in range(2):
        s.op(POOL, lambda e, b=b: e.memset(qh[b][:], 0.0), W=[qh[b]])
        s.op(POOL, lambda e, b=b: e.memset(kh[b][:], 0.0), W=[kh[b]])
        s.op(POOL, lambda e, b=b: e.memset(vh[b][:], 0.0), W=[vh[b]])
    NB = 4
    bank = [kb.ps([128, 512]) for _ in range(NB)]
    Op = [kb.ps([128, 512]) for _ in range(2)]
    oacc = [kb.ps([128, 512]) for _ in range(2)]
    eb = [kb.sb([128, 512], F32) for _ in range(3)]
    spb = [kb.sb([128, 512], BF16) for _ in range(4)]
    t3b = [kb.sb([128, 512], F32) for _ in range(2)]
    ab = [kb.sb([128, 512], BF16) for _ in range(4)]
    carry = [kb.sb([128, 512], F32) for _ in range(2)]
    otb = [kb.sb([64, 512], BF16) for _ in range(2)]

    tiles = []
    for h in range(n_heads):
        for g in range(NG):
            nk = 4 * g + 4
            for ik in range(nk):
                kbk = nk - 1 - ik
                tiles.append((h, g, kbk, ik == 0, kbk == 0, (kbk - 4 * g) if kbk >= 4 * g else -1))

    loaded = set()

    def ensure_head(h):
        if h in loaded or h >= n_heads:
            return
        loaded.add(h)
        b = h % 2
        s.dma(SP, qh[b][0:64, :], qT_d[h * 64:(h + 1) * 64, :], R=[qT_d], W=[qh[b]])
        s.dma(SP, kh[b][0:64, :], kT_d[h * 64:(h + 1) * 64, :], R=[kT_d], W=[kh[b]])
        for c in range(4):
            s.dma(SP, vh[b][:, c * 16:(c + 1) * 16, 0:64],
                  v_d[c * 2048:(c + 1) * 2048, h * 64:(h + 1) * 64].rearrange("(kb p) d -> p kb d", p=128),
                  R=[v_d], W=[vh[b]])

    def stA(i):
        h, g, kbk, first, last, dg = tiles[i]
        ensure_head(h)
        b = h % 2
        bk = bank[i % NB]
        e_ = eb[i % 3]
        sp = spb[i % 4]
        s.op(PE, lambda e: e.matmul(bk[:], lhsT=kh[b][:, kbk * 128:(kbk + 1) * 128], rhs=qh[b][:, g * 512:(g + 1) * 512],
                                    start=True, stop=False), R=[kh[b], qh[b]], W=[bk])
        s.op(ACT, lambda e: e.activation(out=e_[:], in_=bk[:], func=AF.Exp), R=[bk], W=[e_])

    def stA2(i):
        h, g, kbk, first, last, dg = tiles[i]
        e_ = eb[i % 3]
        sp = spb[i % 4]
        s.op(ACT, lambda e: e.activation(out=sp[:], in_=e_[:], func=AF.Ln, bias=1.0), R=[e_], W=[sp])
        if dg >= 0:
            s.op(DVE, lambda e: e.tensor_tensor(out=sp[:], in0=sp[:], in1=masks[:, dg, :], op=ALU.mult),
                 R=[sp, masks], W=[sp])

    def stB(i):
        h, g, kbk, first, last, dg = tiles[i]
        bk = bank[i % NB]
        sp = spb[i % 4]
        o_ = Op[i % 2]
        a_ = ab[i % 4]
        t3 = t3b[i % 2]
        cr = carry[(h * NG + g) % 2]
        s.op(PE, lambda e: e.matmul(bk[:], lhsT=ntri[:], rhs=sp[:], start=False, stop=True), R=[ntri, sp], W=[bk])
        if not last:
            s.op(PE, lambda e: e.matmul(o_[:], lhsT=ones[:], rhs=sp[:], start=True, stop=True), R=[ones, sp], W=[o_])
        if first:
            s.op(ACT, lambda e: e.activation(out=a_[:], in_=bk[:], func=AF.Exp), R=[bk], W=[a_])
            if not last:
                s.op(DVE, lambda e: e.tensor_copy(out=cr[:], in_=o_[:]), R=[o_], W=[cr])
        else:
            s.op(DVE, lambda e: e.tensor_tensor(out=t3[:], in0=bk[:], in1=cr[:], op=ALU.subtract), R=[bk, cr], W=[t3])
            s.op(ACT, lambda e: e.activation(out=a_[:], in_=t3[:], func=AF.Exp), R=[t3], W=[a_])
            if not last:
                s.op(DVE, lambda e: e.tensor_tensor(out=cr[:], in0=o_[:], in1=cr[:], op=ALU.add), R=[o_, cr], W=[cr])
        if dg >= 0:
            s.op(DVE, lambda e: e.tensor_tensor(out=a_[:], in0=a_[:], in1=masks[:, dg, :], op=ALU.mult),
                 R=[a_, masks], W=[a_])

    def stC(i):
        h, g, kbk, first, last, dg = tiles[i]
        b = h % 2
        a_ = ab[i % 4]
        if first and g == 0:
            ensure_head(h + 1)
        oa = oacc[(h * NG + g) % 2]
        s.op(PE, lambda e: e.matmul(oa[:], lhsT=vh[b][:, kbk, :], rhs=a_[:], start=first, stop=last),
             R=[vh[b], a_], W=[oa])
        if last:
            ot = otb[(h * NG + g) % 2]
            s.op(DVE, lambda e: e.tensor_copy(out=ot[:], in_=oa[0:64, :]), R=[oa], W=[ot])
            s.dma(SP, oT_d[h * 64:(h + 1) * 64, g * 512:(g + 1) * 512], ot[:], R=[ot], W=[oT_d])

    n = len(tiles)
    for i in range(n + 3):
        if i < n:
            stA(i)
        if 0 <= i - 1 < n:
            stA2(i - 1)
        if 0 <= i - 2 < n:
            stB(i - 2)
        if 0 <= i - 3 < n:
            stC(i - 3)
    kb.end_stage()


DIL_PAT = ((128, 1), (512, 4), (2048, 16))
DIL_MOFF = (0, 5, 13)
DIL_NMASK = 33


def stage_dil_attn(kb, C, IN, dq_d, dk_d, dv_d, oT_d, n_heads=8):
    s = kb.s
    kb.begin_stage()
    masks = kb.sb([128, DIL_NMASK, 512], BF16)
    for c0 in range(0, DIL_NMASK, 11):
        s.dma(POOL, masks[:, c0:c0 + 11, :], IN["dilmask"][:, c0:c0 + 11, :], R=[IN["dilmask"]], W=[masks])
    shift = kb.sb([128, 64], F32)
    s.dma(SP, shift[:], IN["shift"][:, :], R=[IN["shift"]], W=[shift])
    qh = [kb.sb([128, T], BF16) for _ in range(2)]
    kh = [kb.sb([128, T], BF16) for _ in range(2)]
    vh = [kb.sb([128, 64, 128], BF16) for _ in range(2)]
    for b in range(2):
        s.op(POOL, lambda e, b=b: e.memset(qh[b][:], 0.0), W=[qh[b]])
        s.op(POOL, lambda e, b=b: e.memset(kh[b][:], 0.0), W=[kh[b]])
        s.op(POOL, lambda e, b=b: e.memset(vh[b][:], 1.0), W=[vh[b]])
    acc = kb.sb([128, NG, 512], F32)
    zp = [kb.ps([128, 512]) for _ in range(3)]
    ndp = [kb.ps([128, 512]) for _ in range(2)]
    dnp = [kb.ps([64, 512]) for _ in range(2)]
    pb = [kb.sb([128, 512], BF16) for _ in range(6)]
    rdb = [kb.sb([64, 512], F32) for _ in range(2)]
    otb = [kb.sb([64, 512], BF16) for _ in range(2)]

    units = [(hd, dg) for hd in range(n_heads) for dg in range(3)]
    tiles = []
    for u, (hd, dg) in enumerate(units):
        W_, r_ = DIL_PAT[dg]
        wb = W_ // 128
        for g in range(NG):
            lo = max(0, 4 * g - wb)
            hi = 4 * g + 3
            for kbk in range(lo, hi + 1):
                rel = kbk - (4 * g - wb)
                tiles.append((u, hd, dg, g, kbk, kbk == lo, kbk == hi, DIL_MOFF[dg] + rel))

    loaded = set()

    def ensure_unit(u):
        if u in loaded or u >= len(units):
            return
        loaded.add(u)
        hd, dg = units[u]
        b = u % 2
        r0 = dg * 512 + hd * 64
        s.dma(SP, qh[b][0:64, :], dq_d[r0:r0 + 64, :], R=[dq_d], W=[qh[b]])
        s.dma(SP, kh[b][0:64, :], dk_d[r0:r0 + 64, :], R=[dk_d], W=[kh[b]])
        for c in range(4):
            s.dma(SP, vh[b][:, c * 16:(c + 1) * 16, 0:64],
                  dv_d[c * 2048:(c + 1) * 2048, r0:r0 + 64].rearrange("(kb p) d -> p kb d", p=128),
                  R=[dv_d], W=[vh[b]])

    def stA(i):
        u, hd, dg, g, kbk, first, last, mi = tiles[i]
        ensure_unit(u)
        b = u % 2
        z = zp[i % 3]
        p = pb[i % 6]
        s.op(PE, lambda e: e.matmul(z[:], lhsT=kh[b][:, kbk * 128:(kbk + 1) * 128], rhs=qh[b][:, g * 512:(g + 1) * 512],
                                    start=True, stop=True), R=[kh[b], qh[b]], W=[z])
        s.op(ACT, lambda e: e.activation(out=p[:], in_=z[:], func=AF.Exp), R=[z], W=[p])
        s.op(DVE, lambda e: e.tensor_tensor(out=p[:], in0=p[:], in1=masks[:, mi, :], op=ALU.mult),
             R=[p, masks], W=[p])

    def stB(i):
        u, hd, dg, g, kbk, first, last, mi = tiles[i]
        b = u % 2
        p = pb[i % 6]
        if first and g == 0:
            ensure_unit(u + 1)
        nd = ndp[(u * NG + g) % 2]
        s.op(PE, lambda e: e.matmul(nd[:], lhsT=vh[b][:, kbk, :], rhs=p[:], start=first, stop=last),
             R=[vh[b], p], W=[nd])
        if last:
            if dg == 0:
                s.op(DVE, lambda e: e.tensor_copy(out=acc[:, g, :], in_=nd[:]), R=[nd], W=[acc])
            else:
                s.op(DVE, lambda e: e.tensor_tensor(out=acc[:, g, :], in0=nd[:], in1=acc[:, g, :], op=ALU.add),
                     R=[nd, acc], W=[acc])
            if dg == 2:
                dn = dnp[g % 2]
                rd = rdb[g % 2]
                ot = otb[g % 2]
                s.op(PE, lambda e: e.matmul(dn[:], lhsT=shift[:], rhs=acc[:, g, :], start=True, stop=True),
                     R=[shift, acc], W=[dn])
                s.op(DVE, lambda e: e.reciprocal(out=rd[:], in_=dn[:]), R=[dn], W=[rd])
                s.op(DVE, lambda e: e.tensor_tensor(out=ot[:], in0=acc[0:64, g, :], in1=rd[:], op=ALU.mult),
                     R=[acc, rd], W=[ot])
                s.dma(SP, oT_d[hd * 64:(hd + 1) * 64, g * 512:(g + 1) * 512], ot[:], R=[ot], W=[oT_d])

    n = len(tiles)
    for i in range(n + 3):
        if i < n:
            stA(i)
        if 0 <= i - 3 < n:
            stB(i - 3)
    kb.end_stage()


GDK = 128
GDV = 256


def stage_gla_proj(kb, C, IN, l, x_in, gq_d, gk_d, gks_d, gv_d, gsr_d, gdec_d):
    s = kb.s
    kb.begin_stage()
    alloc_norm_scratch(kb, C)
    NCOL = 3088
    w = kb.sb([128, KC, NCOL], BF16)
    w_ap = IN["gla_w_in"][0, :, :]
    for c0 in range(0, NCOL, 1024):
        c1 = min(NCOL, c0 + 1024)
        s.dma(POOL, w[:, :, c0:c1], w_ap[:, c0:c1].rearrange("(kc p) n -> p kc n", p=128), R=[], W=[w])
    wgu = kb.sb([32, 512], BF16)
    s.dma(POOL, wgu[0:17, :], IN["gla_wgu"][:, :], R=[IN["gla_wgu"]], W=[wgu])
    triblk = kb.sb([128, 128], BF16)
    upblk = kb.sb([128, 128], BF16)
    s.dma(POOL, triblk[:], IN["gla_tri"][:, :], R=[IN["gla_tri"]], W=[triblk])
    s.dma(POOL, upblk[:], IN["gla_up"][:, :], R=[IN["gla_up"]], W=[upblk])
    gd = kb.sb([32, 512], BF16)
    s.op(DVE, lambda e: e.memset(gd[:], 1.0), W=[gd])
    xg = kb.sb([128, KC, 512], F32)
    hT = kb.sb([128, KC, 512], BF16)
    sptok = [kb.sb([128, 512], BF16) for _ in range(4)]
    ef = [kb.sb([128, 512], F32) for _ in range(2)]
    ebf = [kb.sb([128, 512], F32) for _ in range(2)]
    enb = [kb.sb([128, 512], F32) for _ in range(2)]
    ob = [kb.sb([128, 512], BF16) for _ in range(4)]
    dcol = [kb.sb([128, 8], F32) for _ in range(2)]
    pss = [kb.ps([128, 512]) for _ in range(6)]
    a_col = (C["a1"], l * 8)
    sh_col = mod_col(C, l, "sh1")
    st = {"p": 0, "o": 0}

    def nps():
        st["p"] += 1
        return pss[st["p"] % 6]

    def nob():
        st["o"] += 1
        return ob[st["o"] % 4]

    def proj_fm(pst, coff, ncols=128):
        for kc in range(KC):
            s.op(PE, lambda e, kc=kc: e.matmul(pst[0:ncols, :], lhsT=w[:, kc, coff:coff + ncols], rhs=hT[:, kc, :],
                                                start=(kc == 0), stop=(kc == KC - 1)), R=[w, hT], W=[pst])

    def proj_tm(pst, j, coff):
        for kc in range(KC):
            s.op(PE, lambda e, kc=kc: e.matmul(pst[:], lhsT=hT[:, kc, j * 128:(j + 1) * 128],
                                                rhs=w[:, kc, coff:coff + 512],
                                                start=(kc == 0), stop=(kc == KC - 1)), R=[w, hT], W=[pst])

    for g in range(NG):
        tsl = slice(g * 512, (g + 1) * 512)
        s.dma(SP, xg[:], x_in[:, tsl].rearrange("(kc p) t -> p kc t", p=128), R=[x_in], W=[xg])
        norm_group(kb, C, xg, a_col, sh_col, hT)
        p = nps()
        proj_fm(p, 3072, 16)
        s.op(ACT, lambda e: e.activation(out=gd[0:16, :], in_=p[0:16, :], func=AF.Identity), R=[p], W=[gd])
        for j in range(4):
            p = nps()
            e_ = ef[j % 2]
            s.op(PE, lambda e: e.matmul(p[:], lhsT=gd[0:17, j * 128:(j + 1) * 128], rhs=wgu[0:17, :],
                                        start=True, stop=True), R=[gd, wgu], W=[p])
            s.op(ACT, lambda e: e.activation(out=e_[:], in_=p[:], func=AF.Exp, scale=-1.0), R=[p], W=[e_])
            s.op(ACT, lambda e: e.activation(out=sptok[j][:], in_=e_[:], func=AF.Ln, bias=1.0), R=[e_], W=[sptok[j]])
        for h in range(4):
            p = nps()
            for j in range(4):
                s.op(PE, lambda e, j=j: e.matmul(p[:, j * 128:(j + 1) * 128], lhsT=sptok[j][:, h * 128:(h + 1) * 128],
                                                  rhs=triblk[:], start=True, stop=True), R=[sptok[j], triblk], W=[p])
            eb_, en_ = ebf[h % 2], enb[h % 2]
            s.op(ACT, lambda e: e.activation(out=eb_[:], in_=p[:], func=AF.Exp), R=[p], W=[eb_])
            s.op(ACT, lambda e: e.activation(out=en_[:], in_=p[:], func=AF.Exp, scale=-1.0), R=[p], W=[en_])
            dc = dcol[h % 2]
            s.op(POOL, lambda e: e.tensor_copy(out=dc[:], in_=eb_[:].rearrange("p (c t) -> p c t", t=64)[:, :, 63]),
                 R=[eb_], W=[dc])
            s.dma(SP, gdec_d[:, h * 128 + g * 8:h * 128 + (g + 1) * 8], dc[:], R=[dc], W=[gdec_d])
            pq = nps()
            proj_fm(pq, h * 128)
            o = nob()
            s.op(DVE, lambda e: e.scalar_tensor_tensor(out=o[:], in0=pq[:], scalar=float(GDK ** -0.5), in1=eb_[:],
                                                       op0=ALU.mult, op1=ALU.mult), R=[pq, eb_], W=[o])
            s.dma(SP, gq_d[h * 128:(h + 1) * 128, tsl], o[:], R=[o], W=[gq_d])
            pk = nps()
            proj_fm(pk, 512 + h * 128)
            o2 = nob()
            s.op(DVE, lambda e: e.tensor_tensor(out=o2[:], in0=pk[:], in1=en_[:], op=ALU.mult), R=[pk, en_], W=[o2])
            s.dma(SP, gk_d[h * 128:(h + 1) * 128, tsl], o2[:], R=[o2], W=[gk_d])
        for j in range(4):
            pk = nps()
            proj_tm(pk, j, 512)
            pd = nps()
            s.op(PE, lambda e: e.matmul(pd[:], lhsT=upblk[:], rhs=sptok[j][:], start=True, stop=True),
                 R=[upblk, sptok[j]], W=[pd])
            e_ = ef[j % 2]
            s.op(ACT, lambda e: e.activation(out=e_[:], in_=pd[:], func=AF.Exp), R=[pd], W=[e_])
            o = nob()
            s.op(DVE, lambda e: e.tensor_tensor(out=o[:], in0=pk[:], in1=e_[:], op=ALU.mult), R=[pk, e_], W=[o])
            s.dma(SP, gks_d[g * 512 + j * 128:g * 512 + (j + 1) * 128, :], o[:], R=[o], W=[gks_d])
        for j in range(4):
            for c in range(2):
                pv = nps()
                proj_tm(pv, j, 1024 + c * 512)
                o = nob()
                s.op(ACT, lambda e: e.activation(out=o[:], in_=pv[:], func=AF.Identity), R=[pv], W=[o])
                s.dma(SP, gv_d[g * 512 + j * 128:g * 512 + (j + 1) * 128, c * 512:(c + 1) * 512], o[:], R=[o], W=[gv_d])
        for n in range(8):
            pr = nps()
            proj_fm(pr, 2048 + n * 128)
            o = nob()
            s.op(ACT, lambda e: e.activation(out=o[:], in_=pr[:], func=AF.Silu), R=[pr], W=[o])
            s.dma(SP, gsr_d[n * 128:(n + 1) * 128, tsl], o[:], R=[o], W=[gsr_d])
    kb.end_stage()


def stage_gla_scan(kb, C, IN, gq_d, gk_d, gks_d, gv_d, gsr_d, gdec_d, zT_d):
    s = kb.s
    kb.begin_stage()
    ones = C["ones_bf"]
    cmask = kb.sb([128, 128], BF16)
    s.dma(POOL, cmask[:], IN["gla_cmask"][:, :], R=[IN["gla_cmask"]], W=[cmask])
    ng = kb.sb([128, 2], F32)
    s.dma(SP, ng[:], IN["gla_ng_col"][:, :], R=[IN["gla_ng_col"]], W=[ng])
    dec = kb.sb([128, 512], F32)
    s.dma(SP, dec[:], gdec_d[:, :], R=[gdec_d], W=[dec])
    qin = [kb.sb([128, 4, 512], BF16) for _ in range(2)]
    kin = [kb.sb([128, 4, 512], BF16) for _ in range(2)]
    ksb = [kb.sb([128, 4, 512], BF16) for _ in range(2)]
    vb = [kb.sb([128, 4, 1024], BF16) for _ in range(2)]
    srb = [kb.sb([128, 8, 512], BF16) for _ in range(2)]
    ztb = [kb.sb([128, 8, 512], BF16) for _ in range(2)]
    state = [kb.sb([128, GDV], F32) for _ in range(4)]
    sbf = [[kb.sb([128, GDV], BF16) for _ in range(3)] for _ in range(4)]
    for h in range(4):
        s.op(DVE, lambda e, h=h: e.memset(state[h][:], 0.0), W=[state[h]])
        s.op(DVE, lambda e, h=h: e.memset(sbf[h][0][:], 0.0), W=[sbf[h][0]])
    scp = [kb.ps([128, 128]) for _ in range(2)]
    upp = [kb.ps([128, GDV]) for _ in range(2)]
    otp = [kb.ps([128, 128]) for _ in range(2)]
    ssp = [kb.ps([128, 128]) for _ in range(2)]
    scm = [kb.sb([128, 128], BF16) for _ in range(2)]
    sqb = [kb.sb([128, 128], BF16) for _ in range(4)]
    rsb = [kb.sb([128, 128], F32) for _ in range(2)]
    tb = [kb.sb([128, 128], F32) for _ in range(2)]
    ver = [0, 0, 0, 0]
    cnt = {"u": 0, "o": 0, "q": 0, "x": 0}
    for G in range(NG):
        b = G % 2
        tsl = slice(G * 512, (G + 1) * 512)
        s.dma(SP, qin[b][:], gq_d[:, tsl].rearrange("(h p) t -> p h t", p=128), R=[gq_d], W=[qin[b]])
        s.dma(SP, kin[b][:], gk_d[:, tsl].rearrange("(h p) t -> p h t", p=128), R=[gk_d], W=[kin[b]])
        s.dma(SP, ksb[b][:], gks_d[G * 512:(G + 1) * 512, :].rearrange("(j p) c -> p j c", p=128), R=[gks_d], W=[ksb[b]])
        s.dma(SP, vb[b][:], gv_d[G * 512:(G + 1) * 512, :].rearrange("(j p) c -> p j c", p=128), R=[gv_d], W=[vb[b]])
        s.dma(SP, srb[b][:], gsr_d[:, tsl].rearrange("(n p) t -> p n t", p=128), R=[gsr_d], W=[srb[b]])
        Q, Kk, KS, V, SR, ZT = qin[b], kin[b], ksb[b], vb[b], srb[b], ztb[b]
        for j in range(4):
            c0 = 2 * (4 * G + j)
            js = slice(j * 128, (j + 1) * 128)
            for h in range(4):
                cnt["x"] += 1
                x_ = cnt["x"]
                sc = scp[x_ % 2]
                sm = scm[x_ % 2]
                s.op(PE, lambda e: e.matmul(sc[:], lhsT=Kk[:, h, js], rhs=Q[:, h, js], start=True, stop=True),
                     R=[Kk, Q], W=[sc])
                s.op(DVE, lambda e: e.tensor_tensor(out=sm[:], in0=sc[:], in1=cmask[:], op=ALU.mult),
                     R=[sc, cmask], W=[sm])
                S0 = sbf[h][ver[h] % 3]
                S1 = sbf[h][(ver[h] + 1) % 3]
                S2 = sbf[h][(ver[h] + 2) % 3]
                ver[h] += 2
                for ci, (lo, Snew) in enumerate(((0, S1), (64, S2))):
                    cnt["u"] += 1
                    up = upp[cnt["u"] % 2]
                    s.op(PE, lambda e, lo=lo: e.matmul(up[:], lhsT=KS[lo:lo + 64, j, h * 128:(h + 1) * 128],
                                                        rhs=V[lo:lo + 64, j, h * 256:(h + 1) * 256],
                                                        start=True, stop=True), R=[KS, V], W=[up])
                    cc = h * 128 + c0 + ci
                    s.op(DVE, lambda e, cc=cc: e.scalar_tensor_tensor(
                        out=state[h][:], in0=state[h][:], scalar=dec[:, cc:cc + 1], in1=up[:],
                        op0=ALU.mult, op1=ALU.add), R=[state[h], dec, up], W=[state[h]])
                    s.op(ACT, lambda e, Snew=Snew: e.activation(out=Snew[:], in_=state[h][:], func=AF.Identity),
                         R=[state[h]], W=[Snew])
                ots = []
                cnt["q"] += 1
                ss = ssp[cnt["q"] % 2]
                for half in range(2):
                    cnt["o"] += 1
                    ot = otp[cnt["o"] % 2]
                    vs = slice(h * 256 + half * 128, h * 256 + (half + 1) * 128)
                    hs = slice(half * 128, (half + 1) * 128)
                    s.op(PE, lambda e: e.matmul(ot[:], lhsT=V[:, j, vs], rhs=sm[:], start=True, stop=False),
                         R=[V, sm], W=[ot])
                    s.op(PE, lambda e: e.matmul(ot[:, 0:64], lhsT=S0[:, hs], rhs=Q[:, h, j * 128:j * 128 + 64],
                                                start=False, stop=False), R=[S0, Q], W=[ot])
                    s.op(PE, lambda e: e.matmul(ot[:, 64:128], lhsT=S1[:, hs], rhs=Q[:, h, j * 128 + 64:(j + 1) * 128],
                                                start=False, stop=True), R=[S1, Q], W=[ot])
                    sq = sqb[(cnt["o"]) % 4]
                    s.op(ACT, lambda e: e.activation(out=sq[:], in_=ot[:], func=AF.Square, scale=1.0 / 16.0),
                         R=[ot], W=[sq])
                    s.op(PE, lambda e, half=half: e.matmul(ss[:], lhsT=ones[:], rhs=sq[:], start=(half == 0),
                                                            stop=(half == 1)), R=[ones, sq], W=[ss])
                    ots.append(ot)
                rs = rsb[cnt["q"] % 2]
                s.op(ACT, lambda e: e.activation(out=rs[:], in_=ss[:], func=AF.Sqrt, bias=C["eps_col"][:, 0:1]),
                     R=[ss, C["eps_col"]], W=[rs])
                s.op(DVE, lambda e: e.reciprocal(out=rs[:], in_=rs[:]), R=[rs], W=[rs])
                for half in range(2):
                    ot = ots[half]
                    t_ = tb[half]
                    s.op(DVE, lambda e: e.tensor_tensor(out=t_[:], in0=ot[:], in1=rs[:], op=ALU.mult),
                         R=[ot, rs], W=[t_])
                    s.op(DVE, lambda e, half=half: e.scalar_tensor_tensor(
                        out=ZT[:, h * 2 + half, js], in0=t_[:], scalar=ng[:, half:half + 1],
                        in1=SR[:, h * 2 + half, js], op0=ALU.mult, op1=ALU.mult), R=[t_, ng, SR], W=[ZT])
        s.dma(SP, zT_d[:, tsl].rearrange("(n p) t -> p n t", p=128), ZT[:], R=[ZT], W=[zT_d])
    kb.end_stage()


AX = mybir.AxisListType
BIG = 1.0e30


def stage_moe_pre(kb, C, IN, l, x_in, hT_d, gT_d):
    s = kb.s
    kb.begin_stage()
    alloc_norm_scratch(kb, C)
    wr = kb.sb([128, KC, 36], F32)
    rb = kb.sb([128, 36], F32)
    s.dma(SP, wr[:], IN["moe_wr"][l, :, :].rearrange("(kc p) n -> p kc n", p=128), R=[IN["moe_wr"]], W=[wr])
    s.dma(SP, rb[:], IN["moe_rb"][l, :, :], R=[IN["moe_rb"]], W=[rb])
    xb = [kb.sb([128, KC, 512], F32) for _ in range(2)]
    hf = kb.sb([128, KC, 512], F32)
    hb = [kb.sb([128, KC, 512], BF16) for _ in range(2)]
    gts = [kb.sb([32, 512], BF16) for _ in range(2)]
    lgp = [kb.ps([128, 36]) for _ in range(2)]
    gtp = [kb.ps([32, 128]) for _ in range(2)]
    ident = C["ident_f"]

    def small(shape):
        return [kb.sb(shape, F32) for _ in range(2)]

    lgs, gmax, gsel, nmax, ex, gsum, pen = small([128, 36]), small([128, 1]), small([128, 4]), small([128, 1]), small([128, 4]), small([128, 1]), small([128, 4])
    elm, m1, oh1, elm2, m2, oh2 = small([128, 32]), small([128, 1]), small([128, 32]), small([128, 32]), small([128, 1]), small([128, 32])
    dd, ed, w1, w2, gate = small([128, 1]), small([128, 1]), small([128, 1]), small([128, 1]), small([128, 32])
    a_col = (C["a2"], l * 8)
    sh_col = mod_col(C, l, "sh2")
    it = 0
    for g in range(NG):
        xg = xb[g % 2]
        hT = hb[g % 2]
        gt = gts[g % 2]
        tsl = slice(g * 512, (g + 1) * 512)
        s.dma(SP, xg[:], x_in[:, tsl].rearrange("(kc p) t -> p kc t", p=128), R=[x_in], W=[xg])
        norm_group(kb, C, xg, a_col, sh_col, hT, hT_f32=hf)
        s.dma(SP, hT_d[:, tsl].rearrange("(kc p) t -> p kc t", p=128), hT[:], R=[hT], W=[hT_d])
        for j in range(4):
            i = it % 2
            it += 1
            lp = lgp[i]
            for kc in range(KC):
                s.op(PE, lambda e, kc=kc: e.matmul(lp[:], lhsT=hf[:, kc, j * 128:(j + 1) * 128], rhs=wr[:, kc, :],
                                                    start=(kc == 0), stop=(kc == KC - 1)), R=[hf, wr], W=[lp])
            L, GM, GS, NM, EX, GSUM, PEN = lgs[i], gmax[i], gsel[i], nmax[i], ex[i], gsum[i], pen[i]
            ELM, M1, OH1, ELM2, M2, OH2 = elm[i], m1[i], oh1[i], elm2[i], m2[i], oh2[i]
            DD, ED, W1, W2, G = dd[i], ed[i], w1[i], w2[i], gate[i]
            s.op(DVE, lambda e: e.tensor_tensor(out=L[:], in0=lp[:], in1=rb[:], op=ALU.add), R=[lp, rb], W=[L])
            s.op(DVE, lambda e: e.reduce_max(out=GM[:], in_=L[:, 0:4], axis=AX.X), R=[L], W=[GM])
            s.op(DVE, lambda e: e.tensor_scalar(out=GS[:], in0=L[:, 0:4], scalar1=GM[:, 0:1], scalar2=None,
                                                op0=ALU.is_ge), R=[L, GM], W=[GS])
            s.op(DVE, lambda e: e.tensor_scalar(out=NM[:], in0=GM[:], scalar1=-1.0, scalar2=None, op0=ALU.mult),
                 R=[GM], W=[NM])
            s.op(ACT, lambda e: e.activation(out=EX[:], in_=L[:, 0:4], func=AF.Exp, bias=NM[:, 0:1],
                                             accum_out=GSUM[:, 0:1]), R=[L, NM], W=[EX, GSUM])
            s.op(DVE, lambda e: e.reciprocal(out=GSUM[:], in_=GSUM[:]), R=[GSUM], W=[GSUM])
            s.op(DVE, lambda e: e.tensor_scalar(out=PEN[:], in0=GS[:], scalar1=-1.0, scalar2=BIG,
                                                op0=ALU.add, op1=ALU.mult), R=[GS], W=[PEN])
            for gi in range(4):
                s.op(DVE, lambda e, gi=gi: e.tensor_scalar(out=ELM[:, gi * 8:(gi + 1) * 8],
                                                           in0=L[:, 4 + gi * 8:4 + (gi + 1) * 8],
                                                           scalar1=PEN[:, gi:gi + 1], scalar2=None, op0=ALU.add),
                     R=[L, PEN], W=[ELM])
            s.op(DVE, lambda e: e.reduce_max(out=M1[:], in_=ELM[:], axis=AX.X), R=[ELM], W=[M1])
            s.op(DVE, lambda e: e.tensor_scalar(out=OH1[:], in0=ELM[:], scalar1=M1[:, 0:1], scalar2=None,
                                                op0=ALU.is_ge), R=[ELM, M1], W=[OH1])
            s.op(DVE, lambda e: e.scalar_tensor_tensor(out=ELM2[:], in0=OH1[:], scalar=-BIG, in1=ELM[:],
                                                       op0=ALU.mult, op1=ALU.add), R=[OH1, ELM], W=[ELM2])
            s.op(DVE, lambda e: e.reduce_max(out=M2[:], in_=ELM2[:], axis=AX.X), R=[ELM2], W=[M2])
            s.op(DVE, lambda e: e.tensor_scalar(out=OH2[:], in0=ELM2[:], scalar1=M2[:, 0:1], scalar2=None,
                                                op0=ALU.is_ge), R=[ELM2, M2], W=[OH2])
            s.op(DVE, lambda e: e.tensor_tensor(out=DD[:], in0=M2[:], in1=M1[:], op=ALU.subtract), R=[M1, M2], W=[DD])
            s.op(ACT, lambda e: e.activation(out=ED[:], in_=DD[:], func=AF.Exp), R=[DD], W=[ED])
            s.op(DVE, lambda e: e.tensor_scalar(out=W1[:], in0=ED[:], scalar1=1.0, scalar2=None, op0=ALU.add),
                 R=[ED], W=[W1])
            s.op(DVE, lambda e: e.reciprocal(out=W1[:], in_=W1[:]), R=[W1], W=[W1])
            s.op(DVE, lambda e: e.tensor_tensor(out=W2[:], in0=ED[:], in1=W1[:], op=ALU.mult), R=[ED, W1], W=[W2])
            s.op(DVE, lambda e: e.tensor_tensor(out=W1[:], in0=W1[:], in1=GSUM[:], op=ALU.mult), R=[W1, GSUM], W=[W1])
            s.op(DVE, lambda e: e.tensor_tensor(out=W2[:], in0=W2[:], in1=GSUM[:], op=ALU.mult), R=[W2, GSUM], W=[W2])
            s.op(DVE, lambda e: e.tensor_scalar(out=G[:], in0=OH1[:], scalar1=W1[:, 0:1], scalar2=None, op0=ALU.mult),
                 R=[OH1, W1], W=[G])
            s.op(DVE, lambda e: e.scalar_tensor_tensor(out=G[:], in0=OH2[:], scalar=W2[:, 0:1], in1=G[:],
                                                       op0=ALU.mult, op1=ALU.add), R=[OH2, W2, G], W=[G])
            gp = gtp[i]
            s.op(PE, lambda e: e.matmul(gp[:], lhsT=G[:], rhs=ident[:], start=True, stop=True), R=[G, ident], W=[gp])
            s.op(ACT, lambda e, j=j: e.activation(out=gt[:, j * 128:(j + 1) * 128], in_=gp[:], func=AF.Identity),
                 R=[gp], W=[gt])
        s.dma(SP, gT_d[:, tsl], gt[:], R=[gt], W=[gT_d])
    kb.end_stage()


SBK = 2048
NSB = T // SBK


def stage_moe_exp(kb, C, IN, l, x_in, x_out, hT_d, gT_d):
    s = kb.s
    kb.begin_stage()
    sel = kb.sb([128, N_EXP * 128], BF16)
    s.dma(POOL, sel[:], IN["sel"][:, :], R=[IN["sel"]], W=[sel])
    gT = kb.sb([128, SBK], BF16)
    s.op(DVE, lambda e: e.memset(gT[:], 0.0), W=[gT])
    hT = [kb.sb([128, KC, 512], BF16) for _ in range(4)]
    yacc = [kb.sb([128, KC, 512], F32) for _ in range(4)]
    wg = [kb.sb([128, KC, DFF], BF16) for _ in range(2)]
    wu = [kb.sb([128, KC, DFF], BF16) for _ in range(2)]
    wd = [kb.sb([128, 4, D], BF16) for _ in range(2)]
    Ab = [kb.sb([128, 4, 512], BF16) for _ in range(2)]
    Sb = [kb.sb([128, 512], F32) for _ in range(2)]
    Tb = [kb.sb([128, 512], F32) for _ in range(2)]
    xg = kb.sb([128, KC, 512], F32)
    gbps = [kb.ps([128, 512]) for _ in range(1)]
    Gp = [kb.ps([128, 512]) for _ in range(2)]
    Up = [kb.ps([128, 512]) for _ in range(2)]
    Yp = [kb.ps([128, 512]) for _ in range(3)]
    modt = C["mod"]
    g2o = l * 48 + 40
    wi = 0
    ci = 0
    yi = 0
    for sb in range(NSB):
        t0 = sb * SBK
        s.dma(SP, gT[0:32, :], gT_d[:, t0:t0 + SBK], R=[gT_d], W=[gT])
        for g in range(4):
            s.dma(SP, hT[g][:], hT_d[:, t0 + g * 512:t0 + (g + 1) * 512].rearrange("(kc p) t -> p kc t", p=128),
                  R=[hT_d], W=[hT[g]])
        for ex in range(N_EXP):
            b = wi % 2
            wi += 1
            WG, WU, WD = wg[b], wu[b], wd[b]
            s.dma(POOL, WG[:], IN["moe_w_gate"][l, ex, :, :].rearrange("(kc p) f -> p kc f", p=128),
                  R=[IN["moe_w_gate"]], W=[WG])
            s.dma(POOL, WU[:], IN["moe_w_up"][l, ex, :, :].rearrange("(kc p) f -> p kc f", p=128),
                  R=[IN["moe_w_up"]], W=[WU])
            s.dma(POOL, WD[:], IN["moe_w_down"][l, ex, :, :].rearrange("(fc p) n -> p fc n", p=128),
                  R=[IN["moe_w_down"]], W=[WD])

            def gu_phase(g):
                nonlocal ci
                A = Ab[g % 2]
                gbp = gbps[0]
                s.op(PE, lambda e: e.matmul(gbp[:], lhsT=sel[:, ex * 128:(ex + 1) * 128],
                                            rhs=gT[:, g * 512:(g + 1) * 512], start=True, stop=True),
                     R=[sel, gT], W=[gbp])
                for fc in range(4):
                    i = ci % 2
                    ci += 1
                    GP, UP, S_, T_ = Gp[i], Up[i], Sb[i], Tb[i]
                    for kc in range(KC):
                        s.op(PE, lambda e, kc=kc: e.matmul(GP[:], lhsT=WG[:, kc, fc * 128:(fc + 1) * 128],
                                                            rhs=hT[g][:, kc, :], start=(kc == 0), stop=(kc == KC - 1)),
                             R=[WG, hT[g]], W=[GP])
                    for kc in range(KC):
                        s.op(PE, lambda e, kc=kc: e.matmul(UP[:], lhsT=WU[:, kc, fc * 128:(fc + 1) * 128],
                                                            rhs=hT[g][:, kc, :], start=(kc == 0), stop=(kc == KC - 1)),
                             R=[WU, hT[g]], W=[UP])
                    s.op(ACT, lambda e: e.activation(out=S_[:], in_=GP[:], func=AF.Silu), R=[GP], W=[S_])
                    s.op(DVE, lambda e: e.tensor_tensor(out=T_[:], in0=UP[:], in1=S_[:], op=ALU.mult),
                         R=[UP, S_], W=[T_])
                    s.op(DVE, lambda e: e.tensor_tensor(out=A[:, fc, :], in0=gbp[:], in1=T_[:], op=ALU.mult),
                         R=[gbp, T_], W=[A])

            def y_phase(g):
                nonlocal yi
                A = Ab[g % 2]
                for n in range(KC):
                    YP = Yp[yi % 3]
                    yi += 1
                    for fc in range(4):
                        s.op(PE, lambda e, fc=fc: e.matmul(YP[:], lhsT=WD[:, fc, n * 128:(n + 1) * 128],
                                                            rhs=A[:, fc, :], start=(fc == 0), stop=(fc == 3)),
                             R=[WD, A], W=[YP])
                    if ex == 0:
                        s.op(ACT, lambda e: e.activation(out=yacc[g][:, n, :], in_=YP[:], func=AF.Identity),
                             R=[YP], W=[yacc[g]])
                    else:
                        s.op(DVE, lambda e: e.tensor_tensor(out=yacc[g][:, n, :], in0=YP[:], in1=yacc[g][:, n, :],
                                                            op=ALU.add), R=[YP, yacc[g]], W=[yacc[g]])

            gu_phase(0)
            for g in range(1, 4):
                gu_phase(g)
                y_phase(g - 1)
            y_phase(3)
        for g in range(4):
            tsl = slice(t0 + g * 512, t0 + (g + 1) * 512)
            s.dma(SP, xg[:], x_in[:, tsl].rearrange("(kc p) t -> p kc t", p=128), R=[x_in], W=[xg])
            for n in range(KC):
                s.op(DVE, lambda e, n=n: e.scalar_tensor_tensor(
                    out=yacc[g][:, n, :], in0=yacc[g][:, n, :], scalar=modt[:, g2o + n:g2o + n + 1],
                    in1=xg[:, n, :], op0=ALU.mult, op1=ALU.add), R=[yacc[g], modt, xg], W=[yacc[g]])
            s.dma(SP, x_out[:, tsl].rearrange("(kc p) t -> p kc t", p=128), yacc[g][:], R=[yacc[g]], W=[x_out])
    kb.end_stage()


def input_specs(cfg):
    sp = {
        "xT": ([D, T], F32),
        "c_col": ([128, 8], F32),
        "adab_col": ([128, DEPTH * 48], F32),
        "normg_col": ([128, (2 * DEPTH + 1) * 8], F32),
        "ada_w": ([DEPTH, D, 6 * D], F32),
        "ident": ([128, 128], F32),
    }
    if cfg["moe"]:
        sp.update({
            "moe_wr": ([DEPTH, D, 36], F32),
            "moe_rb": ([DEPTH, 128, 36], F32),
            "sel": ([128, N_EXP * 128], F32),
            "moe_w_gate": ([DEPTH, N_EXP, D, DFF], F32),
            "moe_w_up": ([DEPTH, N_EXP, D, DFF], F32),
            "moe_w_down": ([DEPTH, N_EXP, DFF, D], F32),
        })
    if 0 in cfg["mixers"]:
        sp.update({
            "sb_w_in": ([1, D, 3 * D], F32),
            "sb_w_out": ([1, D, D], F32),
            "ntri": ([128, 128], F32),
            "sbmask": ([128, 4, 512], F32),
        })
    if 3 in cfg["mixers"]:
        sp.update({
            "dil_w_in": ([1, D, 4608], F32),
            "dil_w_out": ([1, 512, D], F32),
            "dilmask": ([128, DIL_NMASK, 512], F32),
            "shift": ([128, 64], F32),
        })
    if 1 in cfg["mixers"]:
        sp.update({
            "gla_w_in": ([1, D, 3088], F32),
            "gla_w_out": ([1, D, D], F32),
            "gla_wgu": ([17, 512], F32),
            "gla_tri": ([128, 128], F32),
            "gla_up": ([128, 128], F32),
            "gla_cmask": ([128, 128], F32),
            "gla_ng_col": ([128, 2], F32),
        })
    if 2 in cfg["mixers"]:
        sp.update({
            "conv_w_in": ([1, D, 3 * D], F32),
            "conv_w_col": ([128, 24], F32),
            "conv_b_col": ([128, 8], F32),
            "conv_w_out": ([1, D, D], F32),
        })
    return sp


def build(cfg):
    nc = bass.Bass("TRN2", target_bir_lowering=False)
    IN = {}
    for name, (shape, dt) in input_specs(cfg).items():
        IN[name] = Tl(nc.dram_tensor(name, list(shape), dt, kind="ExternalInput").ap(), multi=True)
    outT = Tl(nc.dram_tensor("outT", [D, T], F32, kind="ExternalOutput").ap(), multi=True)
    with ExitStack() as stack:
        kb = KB(nc, stack)
        C = {}
        xs = [kb.dram("xs0", [D, T], F32), kb.dram("xs1", [D, T], F32)]
        zT = kb.dram("zT", [D, T], BF16)
        hT_d = kb.dram("hT_d", [D, T], BF16)
        qT_d = kb.dram("qT_d", [D, T], BF16)
        kT_d = kb.dram("kT_d", [D, T], BF16)
        v_d = kb.dram("v_d", [T, D], BF16)
        dq_d = kb.dram("dq_d", [1536, T], BF16)
        gq_d = kb.dram("gq_d", [512, T], BF16)
        gk_d = kb.dram("gk_d", [512, T], BF16)
        gks_d = kb.dram("gks_d", [T, 512], BF16)
        gv_d = kb.dram("gv_d", [T, D], BF16)
        gsr_d = kb.dram("gsr_d", [D, T], BF16)
        gdec_d = kb.dram("gdec_d", [128, 512], F32)
        dk_d = kb.dram("dk_d", [1536, T], BF16)
        dv_d = kb.dram("dv_d", [T, 1536], BF16)
        gT_d = kb.dram("gT_d", [N_EXP, T], BF16)
        stage_consts(kb, C, IN)
        stage_mod(kb, C, IN)
        cur = IN["xT"]
        nxt = 0
        for l in range(DEPTH):
            if l in cfg["mixers"]:
                if l == 0:
                    nh = cfg.get("sb_heads", 16)
                    stage_inproj(kb, C, l, cur, IN["sb_w_in"][0, :, :], 3 * D,
                                 [(0, 8, qT_d, 0, 0.125), (D, 8, kT_d, 0, 1.0)], [(2 * D, D, v_d, 0)])
                    stage_sb_attn(kb, C, IN, qT_d, kT_d, v_d, zT, n_heads=nh)
                    stage_outproj(kb, C, l, zT, KC, IN["sb_w_out"][0, :, :], cur, xs[nxt])
                if l == 3:
                    fm, tm = [], []
                    for dg in range(3):
                        fm.append((dg * 1536, 4, dq_d, dg * 512, 0.125))
                        fm.append((dg * 1536 + 512, 4, dk_d, dg * 512, 1.0))
                        tm.append((dg * 1536 + 1024, 512, dv_d, dg * 512))
                    stage_inproj(kb, C, l, cur, IN["dil_w_in"][0, :, :], 4608, fm, tm)
                    stage_dil_attn(kb, C, IN, dq_d, dk_d, dv_d, zT, n_heads=cfg.get("dil_heads", 8))
                    stage_outproj(kb, C, l, zT, 4, IN["dil_w_out"][0, :, :], cur, xs[nxt])
                if l == 1:
                    stage_gla_proj(kb, C, IN, l, cur, gq_d, gk_d, gks_d, gv_d, gsr_d, gdec_d)
                    stage_gla_scan(kb, C, IN, gq_d, gk_d, gks_d, gv_d, gsr_d, gdec_d, zT)
                    stage_outproj(kb, C, l, zT, KC, IN["gla_w_out"][0, :, :], cur, xs[nxt])
                if l == 2:
                    stage_conv(kb, C, IN, l, cur, zT)
                    stage_outproj(kb, C, l, zT, KC, IN["conv_w_out"][0, :, :], cur, xs[nxt])
                cur = xs[nxt]
                nxt ^= 1
            if l in cfg["moe"]:
                stage_moe_pre(kb, C, IN, l, cur, hT_d, gT_d)
                stage_moe_exp(kb, C, IN, l, cur, xs[nxt], hT_d, gT_d)
                cur = xs[nxt]
                nxt ^= 1
        stage_final(kb, C, cur, outT)
        kb.s.wait_all(SP)
        print("instructions:", kb.s.n_ins, "waits:", kb.s.n_wait, "sems:", len(kb.s.sems))
    return nc


def colvec(v):
    return np.ascontiguousarray(v.reshape(KC, 128).T)


def prep_core_inputs(inputs, b, cfg):
    f = np.float32
    m = {}
    m["xT"] = np.ascontiguousarray(inputs["x"][b].T).astype(f, copy=False)
    m["c_col"] = colvec(inputs["c"][b]).astype(f)
    m["adab_col"] = np.ascontiguousarray(
        np.concatenate([inputs["ada_b"][l].reshape(48, 128).T for l in range(DEPTH)], axis=1)).astype(f)
    ng = [colvec(inputs["norm_g"][l, s_]) for l in range(DEPTH) for s_ in range(2)]
    ng.append(colvec(inputs["final_g"]))
    m["normg_col"] = np.ascontiguousarray(np.concatenate(ng, axis=1)).astype(f)
    m["ada_w"] = inputs["ada_w"]
    m["ident"] = np.eye(128, dtype=f)
    if cfg["moe"]:
        m["moe_wr"] = np.ascontiguousarray(np.concatenate([inputs["moe_w_grp"], inputs["moe_w_exp"]], axis=2)).astype(f)
        rb = np.concatenate([inputs["moe_b_grp"], inputs["moe_b_exp"]], axis=1)
        m["moe_rb"] = np.ascontiguousarray(np.broadcast_to(rb[:, None, :], (DEPTH, 128, 36))).astype(f)
        sel = np.zeros((128, N_EXP * 128), dtype=f)
        for e_ in range(N_EXP):
            sel[e_, e_ * 128:(e_ + 1) * 128] = 1.0
        m["sel"] = sel
        m["moe_w_gate"] = inputs["moe_w_gate"]
        m["moe_w_up"] = inputs["moe_w_up"]
        m["moe_w_down"] = inputs["moe_w_down"]
    if 0 in cfg["mixers"]:
        m["sb_w_in"] = inputs["sb_w_in"]
        m["sb_w_out"] = inputs["sb_w_out"]
        jj = np.arange(128)
        m["ntri"] = -(jj[:, None] >= jj[None, :]).astype(f)
        tt = np.arange(512)
        m["sbmask"] = np.ascontiguousarray(np.stack(
            [(tt[None, :] > (128 * i + jj[:, None])).astype(f) for i in range(4)], axis=1))
    if 3 in cfg["mixers"]:
        m["dil_w_in"] = inputs["dil_w_in"]
        m["dil_w_out"] = inputs["dil_w_out"]
        sl = np.arange(128)[:, None]
        tl = np.arange(512)[None, :]
        mk = []
        for (W_, r_) in DIL_PAT:
            for rel in range(W_ // 128 + 4):
                dlt = tl - sl + W_ - 128 * rel
                mk.append(((dlt >= 0) & (dlt <= W_) & (dlt % r_ == 0)).astype(f))
        m["dilmask"] = np.ascontiguousarray(np.stack(mk, axis=1))
        sh = np.zeros((128, 64), dtype=f)
        sh[64 + np.arange(64), np.arange(64)] = 1.0
        m["shift"] = sh
    if 1 in cfg["mixers"]:
        m["gla_w_in"] = inputs["gla_w_in"]
        m["gla_w_out"] = inputs["gla_w_out"]
        m["gla_wgu"] = np.ascontiguousarray(
            np.concatenate([inputs["gla_w_gate_up"][0], inputs["gla_b_gate"][0][None, :]], axis=0)).astype(f)
        ii = np.arange(128)
        same = (ii[:, None] // 64) == (ii[None, :] // 64)
        m["gla_tri"] = (-(1.0 / 16.0) * (same & (ii[:, None] <= ii[None, :]))).astype(f)
        m["gla_up"] = (-(1.0 / 16.0) * (same & (ii[:, None] > ii[None, :]))).astype(f)
        m["gla_cmask"] = (same & (ii[None, :] >= ii[:, None])).astype(f)
        m["gla_ng_col"] = np.ascontiguousarray(inputs["gla_norm_g"][0].reshape(2, 128).T).astype(f)
    if 2 in cfg["mixers"]:
        m["conv_w_in"] = inputs["conv_w_in"]
        m["conv_w_col"] = np.ascontiguousarray(
            np.concatenate([colvec(inputs["conv_w"][0, j]) for j in range(3)], axis=1)).astype(f)
        m["conv_b_col"] = colvec(inputs["conv_b"][0]).astype(f)
        m["conv_w_out"] = inputs["conv_w_out"]
    return m


FULL_CFG = {"mixers": (0, 1, 2, 3), "moe": (0, 1, 2, 3)}


def run(inputs, cfg, n_cores=8, trace=False):
    inputs = {k: np.asarray(v) for k, v in inputs.items()}
    nc = build(cfg)
    in_maps = [prep_core_inputs(inputs, b, cfg) for b in range(n_cores)]
    res = run_bass_kernel_spmd(nc, in_maps, core_ids=list(range(n_cores)), trace=trace)
    out = np.stack([np.ascontiguousarray(r["outT"].T) for r in res.results], axis=0)
    return out.astype(np.float32), res


def kernel(**inputs):
    out, _ = run(inputs, FULL_CFG)
    return out
```
